# Optimizing a Trainium2 kernel written in Bass

```python
import math
import jax
import jax.numpy as jnp
from jax import lax
import numpy as np

D_MODEL = 2048
BATCH = 1
SEQ = 16384
DEPTH = 2

GRID_W = 64
HEAD_DIM = 128
N_FOURIER_GROUPS = 4
FOURIER_W = N_FOURIER_GROUPS * HEAD_DIM
N_NA_HEADS = 8
NA_W = N_NA_HEADS * HEAD_DIM
WIN_H = 8
WIN_W = 16
N_MEM = 256
N_MEM_HEADS = 4
MEM_W = N_MEM_HEADS * HEAD_DIM
N_BRANCHES = 3
IN_W = FOURIER_W + 3 * NA_W + MEM_W + N_BRANCHES * D_MODEL
D_FF = 5632
N_EXPERTS = 8
TOP_K = 2
MOE_BLOCK = 128
EPS = 1e-6

kernel_name = "hybrid_fourier_natten_memory_moe_encoder"


def rms_norm(x, g):
    xf = x.astype(jnp.float32)
    y = xf * lax.rsqrt(jnp.mean(xf * xf, axis=-1, keepdims=True) + EPS)
    return (y * g.astype(jnp.float32)).astype(x.dtype)


def fourier_mix(u):
    b, s, _ = u.shape
    ug = u.astype(jnp.float32).reshape(b, s, N_FOURIER_GROUPS, HEAD_DIM)
    f = jnp.fft.fft2(ug, axes=(1, 3), norm="ortho").real
    return f.reshape(b, s, FOURIER_W).astype(u.dtype)


def neighbourhood_attention(q, k, v, rpb):
    b, s, h, dh = q.shape
    rows = s // GRID_W
    kh = min(WIN_H, rows)
    cols = np.arange(GRID_W)
    col_start = np.clip(cols - WIN_W // 2, 0, GRID_W - WIN_W)
    col_idx = col_start[:, None] + np.arange(WIN_W)[None, :]
    col_off = col_idx - cols[:, None] + (WIN_W - 1)
    q_rows = jnp.moveaxis(q.reshape(b, rows, GRID_W, h, dh), 1, 0)
    k_grid = k.reshape(b, rows, GRID_W, h, dh)
    v_grid = v.reshape(b, rows, GRID_W, h, dh)
    scale = dh ** -0.5

    def row_step(args):
        r, q_r = args
        r_start = jnp.clip(r - kh // 2, 0, rows - kh)
        k_rows = lax.dynamic_slice_in_dim(k_grid, r_start, kh, axis=1)
        v_rows = lax.dynamic_slice_in_dim(v_grid, r_start, kh, axis=1)
        k_win = k_rows[:, :, col_idx]
        v_win = v_rows[:, :, col_idx]
        row_off = r_start + jnp.arange(kh) - r + (WIN_H - 1)
        bias = rpb[:, row_off][:, :, col_off]
        bias = jnp.transpose(bias, (0, 2, 1, 3)).astype(jnp.float32)
        sc = jnp.einsum('bchd,bicjhd->bhcij', q_r, k_win).astype(jnp.float32) * scale
        sc = sc + bias[None]
        p = jax.nn.softmax(sc.reshape(b, h, GRID_W, kh * WIN_W), axis=-1)
        p = p.reshape(b, h, GRID_W, kh, WIN_W).astype(v.dtype)
        return jnp.einsum('bhcij,bicjhd->bchd', p, v_win)

    out = lax.map(row_step, (jnp.arange(rows, dtype=jnp.int32), q_rows))
    return jnp.moveaxis(out, 0, 1).reshape(b, s, h * dh)


def memory_attention(q, mem_h, w_kv, q_g, k_g):
    b, s, _ = q.shape
    m = mem_h.shape[1]
    kv = mem_h @ w_kv
    k, v = jnp.split(kv, 2, axis=-1)
    q = rms_norm(q.reshape(b, s, N_MEM_HEADS, HEAD_DIM), q_g)
    k = rms_norm(k.reshape(b, m, N_MEM_HEADS, HEAD_DIM), k_g)
    v = v.reshape(b, m, N_MEM_HEADS, HEAD_DIM)
    sc = jnp.einsum('bshd,bmhd->bhsm', q, k).astype(jnp.float32) * (HEAD_DIM ** -0.5)
    p = jax.nn.softmax(sc, axis=-1).astype(v.dtype)
    o = jnp.einsum('bhsm,bmhd->bshd', p, v)
    return o.reshape(b, s, MEM_W)


def swiglu(h, w_gate, w_up, w_down):
    return (jax.nn.silu(h @ w_gate) * (h @ w_up)) @ w_down


def moe_swiglu(h, w_router, w_gate, w_up, w_down):
    b, s, d = h.shape
    n = b * s
    hf = h.reshape(n, d)
    logits = (hf @ w_router).astype(jnp.float32)
    top_logit, top_e = lax.top_k(logits, TOP_K)
    gates = jax.nn.softmax(top_logit, axis=-1)
    e_flat = top_e.reshape(-1)
    t_flat = jnp.repeat(jnp.arange(n, dtype=jnp.int32), TOP_K)
    w_flat = gates.reshape(-1)
    order = jnp.argsort(e_flat)
    e_sorted = e_flat[order]
    counts = jnp.bincount(e_flat, length=N_EXPERTS)
    starts = jnp.cumsum(counts) - counts
    padded = (counts + MOE_BLOCK - 1) // MOE_BLOCK * MOE_BLOCK
    pad_ends = jnp.cumsum(padded)
    pad_starts = pad_ends - padded
    n_assign = n * TOP_K
    dest = pad_starts[e_sorted] + jnp.arange(n_assign) - starts[e_sorted]
    cap = n_assign + N_EXPERTS * MOE_BLOCK
    n_blocks = cap // MOE_BLOCK
    tok_buf = jnp.full((cap,), n, jnp.int32).at[dest].set(t_flat[order])
    wt_buf = jnp.zeros((cap,), jnp.float32).at[dest].set(w_flat[order])
    blk_e = jnp.clip(jnp.searchsorted(pad_ends, jnp.arange(n_blocks) * MOE_BLOCK, side='right'),
                     0, N_EXPERTS - 1)
    x_pad = jnp.concatenate([hf, jnp.zeros((1, d), hf.dtype)], axis=0)
    xb = x_pad[tok_buf].reshape(n_blocks, MOE_BLOCK, d)

    def expert_block(args):
        xblk, e = args
        return swiglu(xblk, w_gate[e], w_up[e], w_down[e])

    yb = lax.map(expert_block, (xb, blk_e)).reshape(cap, d)
    y = jnp.zeros((n + 1, d), jnp.float32).at[tok_buf].add(yb.astype(jnp.float32) * wt_buf[:, None])
    return y[:n].reshape(b, s, d).astype(h.dtype)


def setup_inputs(seed: int = 0) -> dict:
    key = jax.random.key(seed)
    ks = jax.random.split(key, 24)
    L = DEPTH
    n_dense = (DEPTH + 1) // 2
    n_moe = DEPTH // 2
    f32 = jnp.float32

    def nrm(k, shape, scale):
        return jax.random.normal(k, shape, f32) * scale

    def gain(k, shape):
        return 1.0 + 0.02 * jax.random.normal(k, shape, f32)

    return {
        "x": nrm(ks[0], (BATCH, SEQ, D_MODEL), 1.0),
        "mem": nrm(ks[1], (BATCH, N_MEM, D_MODEL), 1.0),
        "ln_mix_g": gain(ks[2], (L, D_MODEL)),
        "w_in": nrm(ks[3], (L, D_MODEL, IN_W), D_MODEL ** -0.5),
        "na_q_g": gain(ks[4], (L, HEAD_DIM)),
        "na_k_g": gain(ks[5], (L, HEAD_DIM)),
        "na_rpb": nrm(ks[6], (L, N_NA_HEADS, 2 * WIN_H - 1, 2 * WIN_W - 1), 0.02),
        "mem_ln_g": gain(ks[7], (L, D_MODEL)),
        "w_mem_kv": nrm(ks[8], (L, D_MODEL, 2 * MEM_W), D_MODEL ** -0.5),
        "mem_q_g": gain(ks[9], (L, HEAD_DIM)),
        "mem_k_g": gain(ks[10], (L, HEAD_DIM)),
        "w_fourier_out": nrm(ks[11], (L, FOURIER_W, D_MODEL), FOURIER_W ** -0.5),
        "w_na_out": nrm(ks[12], (L, NA_W, D_MODEL), NA_W ** -0.5),
        "w_mem_out": nrm(ks[13], (L, MEM_W, D_MODEL), MEM_W ** -0.5),
        "w_o": nrm(ks[14], (L, D_MODEL, D_MODEL), D_MODEL ** -0.5),
        "ln_ffn_g": gain(ks[15], (L, D_MODEL)),
        "ffn_w_gate": nrm(ks[16], (n_dense, D_MODEL, D_FF), D_MODEL ** -0.5),
        "ffn_w_up": nrm(ks[17], (n_dense, D_MODEL, D_FF), D_MODEL ** -0.5),
        "ffn_w_down": nrm(ks[18], (n_dense, D_FF, D_MODEL), D_FF ** -0.5),
        "moe_router": nrm(ks[19], (n_moe, D_MODEL, N_EXPERTS), D_MODEL ** -0.5),
        "moe_w_gate": nrm(ks[20], (n_moe, N_EXPERTS, D_MODEL, D_FF), D_MODEL ** -0.5),
        "moe_w_up": nrm(ks[21], (n_moe, N_EXPERTS, D_MODEL, D_FF), D_MODEL ** -0.5),
        "moe_w_down": nrm(ks[22], (n_moe, N_EXPERTS, D_FF, D_MODEL), D_FF ** -0.5),
    }


def reference(x, mem, ln_mix_g, w_in, na_q_g, na_k_g, na_rpb, mem_ln_g, w_mem_kv,
              mem_q_g, mem_k_g, w_fourier_out, w_na_out, w_mem_out, w_o, ln_ffn_g,
              ffn_w_gate, ffn_w_up, ffn_w_down, moe_router, moe_w_gate, moe_w_up,
              moe_w_down):
    b, s, d = x.shape
    splits = [FOURIER_W,
              FOURIER_W + NA_W,
              FOURIER_W + 2 * NA_W,
              FOURIER_W + 3 * NA_W,
              FOURIER_W + 3 * NA_W + MEM_W]
    for l in range(DEPTH):
        h = rms_norm(x, ln_mix_g[l])
        proj = h @ w_in[l]
        f_in, q_na, k_na, v_na, q_mem, gate_logits = jnp.split(proj, splits, axis=-1)

        o_f = fourier_mix(f_in) @ w_fourier_out[l]

        q_na = rms_norm(q_na.reshape(b, s, N_NA_HEADS, HEAD_DIM), na_q_g[l])
        k_na = rms_norm(k_na.reshape(b, s, N_NA_HEADS, HEAD_DIM), na_k_g[l])
        v_na = v_na.reshape(b, s, N_NA_HEADS, HEAD_DIM)
        o_n = neighbourhood_attention(q_na, k_na, v_na, na_rpb[l]) @ w_na_out[l]

        mem_h = rms_norm(mem, mem_ln_g[l])
        o_m = memory_attention(q_mem, mem_h, w_mem_kv[l], mem_q_g[l], mem_k_g[l]) @ w_mem_out[l]

        g = jax.nn.sigmoid(gate_logits.astype(jnp.float32)).astype(x.dtype)
        g = g.reshape(b, s, N_BRANCHES, d)
        merged = g[:, :, 0] * o_f + g[:, :, 1] * o_n + g[:, :, 2] * o_m
        x = x + merged @ w_o[l]

        h2 = rms_norm(x, ln_ffn_g[l])
        if l % 2 == 0:
            i = l // 2
            y = swiglu(h2, ffn_w_gate[i], ffn_w_up[i], ffn_w_down[i])
        else:
            i = l // 2
            y = moe_swiglu(h2, moe_router[i], moe_w_gate[i], moe_w_up[i], moe_w_down[i])
        x = x + y
    return x
```

```python
import math
from contextlib import ExitStack
import numpy as np
import ml_dtypes
import concourse.bass as bass
import concourse.mybir as mybir
from concourse.bass_utils import run_bass_kernel_spmd

F32 = mybir.dt.float32
BF16 = mybir.dt.bfloat16
ALU = mybir.AluOpType
AF = mybir.ActivationFunctionType

NCORES = 8
D = 2048
KC = 16
T = 2048
TE = 2560
DFF = 5632
NFF = 44
NEG = -30000.0
EPS = 1e-6


class Buf:
    __slots__ = ("name", "last_w", "readers")

    def __init__(self, name):
        self.name = name
        self.last_w = None
        self.readers = []


class Op:
    __slots__ = ("eng", "fn", "waits", "needs_inc", "is_dma", "sem", "sem_val", "milestone")

    def __init__(self, eng, fn, is_dma=False):
        self.eng = eng
        self.fn = fn
        self.waits = []
        self.needs_inc = False
        self.is_dma = is_dma
        self.sem = None
        self.sem_val = 0
        self.milestone = 0


ENGS = ("pe", "dve", "act", "pool", "sp")


class Prog:
    def __init__(self, nc, n_dma_sems=32):
        self.nc = nc
        self.q = {e: [] for e in ENGS}
        self.n_dma_sems = n_dma_sems
        self.dma_count = 0
        self.dma_last = [None] * n_dma_sems
        self.dma_cnt_per = [0] * n_dma_sems
        self.nbuf = 0
        self.cc_ops = []

    def buf(self, name=None):
        self.nbuf += 1
        return Buf(name or f"b{self.nbuf}")

    def bufs(self, n, name="b"):
        return [self.buf(f"{name}{i}") for i in range(n)]

    def _dep(self, op, d):
        if d is None or d is op:
            return
        if (not d.is_dma) and d.eng == "pe" and op.eng == "pe" and not op.is_dma:
            return
        if not d.is_dma:
            d.needs_inc = True
        op.waits.append(d)

    def op(self, eng, fn, reads=(), writes=(), is_dma=False):
        o = Op(eng, fn, is_dma)
        for r in reads:
            self._dep(o, r.last_w)
        for w in writes:
            self._dep(o, w.last_w)
            for rd in w.readers:
                self._dep(o, rd)
        for w in writes:
            w.last_w = o
            w.readers = []
        for r in reads:
            r.readers.append(o)
        if is_dma:
            s = self.dma_count % self.n_dma_sems
            self.dma_count += 1
            prev = self.dma_last[s]
            if prev is not None:
                o.waits.append(prev)
            self.dma_cnt_per[s] += 1
            o.sem = s
            o.sem_val = 16 * self.dma_cnt_per[s]
            self.dma_last[s] = o
        self.q[eng].append(o)
        return o

    def dma(self, eng, out, in_, reads=(), writes=(), **kw):
        def fn(e):
            return e.dma_start(out=out, in_=in_, **kw)
        return self.op(eng, fn, reads, writes, is_dma=True)

    def cc(self, kind, ins, outs, reads=(), writes=()):
        def fn(e):
            return e.collective_compute(kind, ALU.bypass, replica_groups=[list(range(NCORES))], ins=ins, outs=outs)
        o = Op("pool", fn, True)
        for r in reads:
            self._dep(o, r.last_w)
        for w in writes:
            self._dep(o, w.last_w)
            for rd in w.readers:
                self._dep(o, rd)
        for w in writes:
            w.last_w = o
            w.readers = []
        for r in reads:
            r.readers.append(o)
        self.cc_ops.append(o)
        o.sem = -len(self.cc_ops)
        o.sem_val = 1
        self.q["pool"].append(o)
        return o

    def barrier(self):
        lasts = []
        for e in ENGS:
            for o in reversed(self.q[e]):
                if not o.is_dma:
                    lasts.append(o)
                    break
        dl = [d for d in self.dma_last if d is not None] + list(self.cc_ops)
        for e in ENGS:
            o = Op(e, lambda eng: eng.nop())
            for d in lasts:
                if d.eng == e:
                    continue
                d.needs_inc = True
                o.waits.append(d)
            for d in dl:
                o.waits.append(d)
            self.q[e].append(o)

    def emit(self, final_wait_ops=()):
        nc = self.nc
        for e in ENGS:
            c = 0
            for o in self.q[e]:
                if o.is_dma:
                    continue
                if o.needs_inc:
                    c += 1
                    o.milestone = c
        with ExitStack() as st:
            esem = {e: st.enter_context(nc.semaphore(f"s_{e}")) for e in ENGS}
            dsem = [st.enter_context(nc.semaphore(f"d_{i}")) for i in range(self.n_dma_sems)]
            csem = [st.enter_context(nc.semaphore(f"c_{i}")) for i in range(len(self.cc_ops))]
            block = st.enter_context(nc.Block())
            prog = self

            def run(engname, eng):
                waited = {}
                for o in prog.q[engname]:
                    for d in o.waits:
                        if d.is_dma:
                            key = ("d", d.sem)
                            val = d.sem_val
                            sem = dsem[d.sem] if d.sem >= 0 else csem[-d.sem - 1]
                        else:
                            key = ("e", d.eng)
                            val = d.milestone
                            sem = esem[d.eng]
                        if waited.get(key, 0) >= val:
                            continue
                        waited[key] = val
                        eng.wait_ge(sem, val)
                    ins = o.fn(eng)
                    if o.is_dma and o.sem < 0:
                        ins.then_inc(csem[-o.sem - 1])
                    elif o.is_dma:
                        ins.then_inc(dsem[o.sem], 16)
                    elif o.needs_inc:
                        ins.then_inc(esem[engname], 1)
                if engname == "sp":
                    for d in final_wait_ops:
                        eng.wait_ge(dsem[d.sem], d.sem_val)

            @block.tensor
            def _(eng):
                run("pe", eng)

            @block.vector
            def _(eng):
                run("dve", eng)

            @block.scalar
            def _(eng):
                run("act", eng)

            @block.gpsimd
            def _(eng):
                run("pool", eng)

            @block.sync
            def _(eng):
                run("sp", eng)


class Arena:
    def __init__(self, ap32, n32):
        self.ap = ap32
        self.n = n32
        self.off = 0

    def reset(self):
        self.off = 0

    def alloc(self, shape, dt):
        ne = 1
        for s in shape:
            ne *= s
        sz = 2 if dt == BF16 else 4
        n32 = (ne * sz + 3) // 4
        n32 = (n32 + 7) // 8 * 8
        assert self.off + n32 <= self.n, f"arena overflow {self.off}+{n32}>{self.n}"
        a = self.ap[:, self.off:self.off + n32]
        self.off += n32
        if dt != F32:
            a = a.bitcast(dt)
        a = a[:, :ne]
        if len(shape) == 2:
            a = a.rearrange("p (a b) -> p a b", b=shape[1])
        elif len(shape) == 3:
            a = a.rearrange("p (a b c) -> p a b c", b=shape[1], c=shape[2])
        return a


class K:
    def __init__(self, nc, st):
        self.nc = nc
        self.p = Prog(nc)
        N32 = 49152
        big = st.enter_context(nc.sbuf_tensor("arena", [128, N32], F32))
        self.ar = Arena(big, N32)
        cst = st.enter_context(nc.sbuf_tensor("consts", [128, 1024], F32))
        self.car = Arena(cst, 1024)
        self.ps = [st.enter_context(nc.psum_tensor(f"ps{i}", [128, 512], F32)) for i in range(8)]
        self.Bps = self.p.bufs(8, "ps")
        self.Bc = self.p.buf("consts")
        self.rr = 0

    def phase(self):
        self.p.barrier()
        self.ar.reset()

    def ev_eng(self):
        self.rr += 1
        return "act" if self.rr % 2 else "dve"

    def copy(self, eng, out, in_, reads, writes):
        if eng == "act":
            return self.p.op("act", lambda e: e.copy(out=out, in_=in_), reads, writes)
        return self.p.op(eng, lambda e: e.tensor_copy(out=out, in_=in_), reads, writes)

    def consts(self):
        p = self.p
        c = self.car
        self.identf = c.alloc([128], F32)
        self.ident = c.alloc([128], BF16)
        self.ones = c.alloc([128], BF16)
        identf, ident, ones = self.identf, self.ident, self.ones

        def mk(e):
            e.memset(identf, 0.0)
            return e.affine_select(out=identf, in_=identf, pattern=[[-1, 128]], compare_op=ALU.not_equal,
                                   fill=1.0, base=0, channel_multiplier=1)
        p.op("pool", mk, writes=[self.Bc])
        p.op("dve", lambda e: e.tensor_copy(out=ident, in_=identf), reads=[self.Bc], writes=[self.Bc])
        p.op("dve", lambda e: e.memset(ones, 1.0), writes=[self.Bc])
        self.epsc = c.alloc([1], F32)
        epsc = self.epsc
        p.op("dve", lambda e: e.memset(epsc, EPS), writes=[self.Bc])

    def norm_tiles(self, src_rows, g_row, hT, tiles, Bh, keep=None, router=None):
        p, ar = self.p, self.ar
        gbc = ar.alloc([D], F32)
        Bg = p.buf()
        p.dma("sp", gbc, g_row.partition_broadcast(128), writes=[Bg])
        xt = [ar.alloc([D], F32) for _ in range(2)]
        Bx = p.bufs(2, "xt")
        junk = ar.alloc([D], BF16)
        Bj = p.buf()
        hb = [ar.alloc([D], BF16) for _ in range(2)]
        Bhb = p.bufs(2, "hb")
        ss = [ar.alloc([1], F32) for _ in range(2)]
        Bss = p.bufs(2, "ss")
        if router is not None:
            h2f = None
            Bh2f = None
            hTf = ar.alloc([4, 128], F32)
            BhTf = p.buf()
        pT = [self.ps[6][:].bitcast(BF16), self.ps[7][:].bitcast(BF16)]
        BpT = [self.Bps[6], self.Bps[7]]
        for n, (i, dcol) in enumerate(tiles):
            b = n % 2
            p.dma("sp", xt[b], src_rows(i), writes=[Bx[b]])
            if keep is not None:
                keep(n, xt[b], Bx[b])
            p.op("act", lambda e, b=b: e.activation(out=junk, in_=xt[b], func=AF.Square, accum_out=ss[b]),
                 reads=[Bx[b]], writes=[Bj, Bss[b]])
            p.op("act", lambda e, b=b: e.activation(out=ss[b], in_=ss[b], func=AF.Ln, bias=self.epsc, scale=1.0 / D),
                 reads=[Bss[b], self.Bc], writes=[Bss[b]])
            p.op("act", lambda e, b=b: e.activation(out=ss[b], in_=ss[b], func=AF.Exp, scale=-0.5),
                 reads=[Bss[b]], writes=[Bss[b]])
            if router is None:
                p.op("dve", lambda e, b=b: e.scalar_tensor_tensor(out=hb[b], in0=xt[b], scalar=ss[b], in1=gbc,
                                                                  op0=ALU.mult, op1=ALU.mult),
                     reads=[Bx[b], Bss[b], Bg], writes=[Bhb[b]])
            else:
                h2f = xt[b]
                Bh2f = Bx[b]
                p.op("dve", lambda e, b=b: e.scalar_tensor_tensor(out=xt[b], in0=xt[b], scalar=ss[b], in1=gbc,
                                                                  op0=ALU.mult, op1=ALU.mult),
                     reads=[Bss[b], Bg], writes=[Bx[b]])
                p.op("act", lambda e, b=b: e.copy(out=hb[b], in_=xt[b]), reads=[Bx[b]], writes=[Bhb[b]])
                wr, Bwr, lg_ps, Blg, done = router
                for g4 in range(4):
                    def trf(e, g4=g4, h2f=h2f):
                        r = None
                        for j in range(4):
                            kc = g4 * 4 + j
                            r = e.transpose(self.ps[5][:, j * 128:(j + 1) * 128], h2f[:, kc * 128:(kc + 1) * 128],
                                            self.identf)
                        return r
                    p.op("pe", trf, reads=[Bh2f, self.Bc], writes=[self.Bps[5]])
                    p.op("dve", lambda e: e.tensor_copy(out=hTf, in_=self.ps[5][:].rearrange("p (a b) -> p a b", b=128)),
                         reads=[self.Bps[5]], writes=[BhTf])

                    def lgm(e, g4=g4):
                        r = None
                        for j in range(4):
                            kc = g4 * 4 + j
                            r = e.matmul(lg_ps, lhsT=hTf[:, j, :], rhs=wr[:, kc, :], start=(kc == 0), stop=(kc == 15))
                        return r
                    p.op("pe", lgm, reads=[BhTf, Bwr], writes=[Blg])
                done(n)
            for half in range(2):
                def tr(e, b=b, half=half):
                    r = None
                    for j in range(8):
                        kc = half * 8 + j
                        r = e.transpose(pT[half][:, j * 128:(j + 1) * 128], hb[b][:, kc * 128:(kc + 1) * 128], self.ident)
                    return r
                p.op("pe", tr, reads=[Bhb[b], self.Bc], writes=[BpT[half]])
                self.copy("act" if half else "dve", hT[:, half * 8:(half + 1) * 8, dcol:dcol + 128],
                          pT[half].rearrange("p (a b) -> p a b", b=128), [BpT[half]], [Bh])

    def proj_F(self, w_dram_cols, ncols, hT, Bh, col0, ntok, evac, wblk=512, psb=(0, 1, 2, 3), wbufs=None):
        p, ar = self.p, self.ar
        if wbufs is None:
            wb = [ar.alloc([KC, wblk], BF16) for _ in range(2)]
            Bw = p.bufs(2, "w")
        else:
            wb, Bw = wbufs
        nblk = (ncols + wblk - 1) // wblk
        k = 0
        for bi in range(nblk):
            b = bi % 2
            nc_ = min(wblk, ncols - bi * wblk)
            p.dma("pool", wb[b][:, :, :nc_], w_dram_cols(bi * wblk, nc_).rearrange("(c p) n -> p c n", p=128),
                  writes=[Bw[b]])
            for c in range(nc_ // 128):
                for t0 in range(0, ntok, 512):
                    nt = min(512, ntok - t0)
                    pi = psb[k % len(psb)]
                    k += 1

                    def mm(e, b=b, c=c, t0=t0, nt=nt, pi=pi):
                        r = None
                        for kc in range(KC):
                            r = e.matmul(self.ps[pi][:, :nt], lhsT=wb[b][:, kc, c * 128:(c + 1) * 128],
                                         rhs=hT[:, kc, col0 + t0:col0 + t0 + nt], start=(kc == 0), stop=(kc == KC - 1))
                        return r
                    p.op("pe", mm, reads=[Bw[b], Bh], writes=[self.Bps[pi]])
                    evac(bi * (wblk // 128) + c, t0, nt, self.ps[pi][:, :nt], self.Bps[pi])

    def qknorm(self, ps_ap, Bp, n, gvec, Bgv, out, Bout, scr):
        p = self.p
        sq, Bsq, rr, Brr, ssb = scr
        p.op("act", lambda e: e.activation(out=sq[:, :n], in_=ps_ap, func=AF.Square), reads=[Bp], writes=[Bsq])
        p.op("pe", lambda e: e.matmul(self.ps[ssb][:, :n], lhsT=self.ones, rhs=sq[:, :n], start=True, stop=True),
             reads=[Bsq, self.Bc], writes=[self.Bps[ssb]])
        p.op("act", lambda e: e.activation(out=rr[:, :n], in_=self.ps[ssb][:, :n], func=AF.Ln, bias=self.epsc, scale=1.0 / 128),
             reads=[self.Bps[ssb], self.Bc], writes=[Brr])
        p.op("act", lambda e: e.activation(out=rr[:, :n], in_=rr[:, :n], func=AF.Exp, scale=-0.5),
             reads=[Brr], writes=[Brr])
        p.op("dve", lambda e: e.scalar_tensor_tensor(out=out, in0=ps_ap, scalar=gvec, in1=rr[:, :n],
                                                     op0=ALU.mult, op1=ALU.mult),
             reads=[Bp, Brr, Bgv], writes=[Bout])

    def qk_scratch(self, ssb):
        ar, p = self.ar, self.p
        return (ar.alloc([512], BF16), p.buf(), ar.alloc([512], F32), p.buf(), ssb)

    def load_gvec(self, g_dram_row, scale):
        p = self.p
        gv = self.car.alloc([1], F32)
        B = p.buf()
        p.dma("sp", gv, g_dram_row.rearrange("(p o) -> p o", o=1), writes=[B])
        if scale != 1.0:
            p.op("dve", lambda e: e.tensor_scalar(out=gv, in0=gv, scalar1=scale, scalar2=None, op0=ALU.mult),
                 reads=[B], writes=[B])
        return gv, B


def phase_fin(k, src_rows, g, wcols, cs, z, own_off, do_norm, hT, Bh):
    p, ar = k.p, k.ar
    if do_norm:
        hT = ar.alloc([KC, T], BF16)
        Bh = p.buf("hT")
        k.norm_tiles(src_rows, g, hT, [(i, i * 128) for i in range(16)], Bh)
        own_off = 0
    finT = ar.alloc([4, T], BF16)
    Bf = p.buf("finT")
    csb = ar.alloc([256], BF16)
    Bcs = p.buf()
    p.dma("pool", csb, cs, writes=[Bcs])

    def ev(ci, t0, nt, ps_ap, Bp):
        k.copy(k.ev_eng(), finT[:, ci, t0:t0 + nt], ps_ap, [Bp], [Bf])
    k.proj_F(wcols, 512, hT, Bh, own_off, T, ev)
    zs = [ar.alloc([4, 256], BF16) for _ in range(2)]
    Bz = p.bufs(2, "zs")
    outs = []
    for i in range(16):
        b = i % 2
        pz = [k.ps[4 + 2 * b][:], k.ps[5 + 2 * b][:]]
        Bpz = [k.Bps[4 + 2 * b], k.Bps[5 + 2 * b]]
        for hh in range(2):
            def mm(e, i=i, hh=hh, pz=pz):
                r = None
                for gg in range(2):
                    g_ = hh * 2 + gg
                    r = e.matmul(pz[hh][:, gg * 256:(gg + 1) * 256], lhsT=finT[:, g_, i * 128:(i + 1) * 128], rhs=csb,
                                 start=True, stop=True)
                return r
            p.op("pe", mm, reads=[Bf, Bcs], writes=[Bpz[hh]])
            k.copy("act" if hh else "dve", zs[b][:, hh * 2:(hh + 1) * 2, :],
                   pz[hh].rearrange("p (a b) -> p a b", b=256), [Bpz[hh]], [Bz[b]])
        outs.append(p.dma("sp", z[:, i * 128:(i + 1) * 128, :].rearrange("g t c -> t g c"), zs[b], reads=[Bz[b]]))
    return outs


def emit_layer(k, nc, layer, x, zall, W, out_dma):
    moe = (layer % 2 == 1)
    p, ar = k.p, k.ar
    mem = W["mem"]; ln_mix_g = W["ln_mix_g"]; w_in = W["w_in"]; na_q_g = W["na_q_g"]; na_k_g = W["na_k_g"]
    ttab = W["ttab"]; rowmask = W["rowmask"]; mem_ln_g = W["mem_ln_g"]; w_mem_kv = W["w_mem_kv"]
    mem_q_g = W["mem_q_g"]; mem_k_g = W["mem_k_g"]; w_f_out = W["w_f_out"]; w_na_out = W["w_na_out"]
    w_mem_out = W["w_mem_out"]; w_o = W["w_o"]; ln_ffn_g = W["ln_ffn_g"]; w128 = W["w128"]; mtab = W["mtab"]
    wg_d = W["wg"]; wu_d = W["wu"]; wd_d = W["wd"]
    NE = (_DBG_NE or 8) if moe else 1
    if moe:
        wr_d = W["wr"]
    ft_d = W["ft_d"]; na_d = W["na_d"]; mo_d = W["mo_d"]; gates_d = W["gates_d"]; xmid_d = W["xmid_d"]
    SC = 1.0 / math.sqrt(128.0)

    k.phase()
    hT = ar.alloc([KC, TE], BF16)
    Bh = p.buf("hT")
    mark_h = ar.off
    k.norm_tiles(lambda i: x[i * 128:(i + 1) * 128, :], ln_mix_g, hT, [(i, i * 128) for i in range(20)], Bh)

    k.phase()
    ar.off = mark_h
    gs = [ar.alloc([512], F32) for _ in range(4)]
    Bgs = p.bufs(4, "gs")
    cnt = [0]

    def ev_g(ci, t0, nt, ps_ap, Bp):
        b = cnt[0] % 4
        cnt[0] += 1
        p.op("act", lambda e: e.activation(out=gs[b][:, :nt], in_=ps_ap, func=AF.Sigmoid), reads=[Bp], writes=[Bgs[b]])
        p.dma("sp", gates_d[ci, :, t0:t0 + nt], gs[b][:, :nt], reads=[Bgs[b]])
    k.proj_F(lambda c0, n: w_in[:, 4096 + c0:4096 + c0 + n], 6144, hT, Bh, 256, T, ev_g)

    k.phase()
    ar.off = mark_h
    mhT = ar.alloc([KC, 256], BF16)
    Bmh = p.buf("mhT")
    mark_m = ar.off
    k.norm_tiles(lambda i: mem[i * 128:(i + 1) * 128, :], mem_ln_g, mhT, [(0, 0), (1, 128)], Bmh)
    k.phase()
    ar.off = mark_m
    gq_m, Bgqm = k.load_gvec(mem_q_g, SC)
    gk_m, Bgkm = k.load_gvec(mem_k_g, 1.0)
    kTm = ar.alloc([4, 256], BF16)
    BkTm = p.buf("kTm")
    vm = ar.alloc([2, 512], BF16)
    Bvm = p.buf("vm")
    scr = k.qk_scratch(4)

    def ev_km(ci, t0, nt, ps_ap, Bp):
        k.qknorm(ps_ap, Bp, nt, gk_m, Bgkm, kTm[:, ci, t0:t0 + nt], BkTm, scr)
    k.proj_F(lambda c0, n: w_mem_kv[:, c0:c0 + n], 512, mhT, Bmh, 0, 256, ev_km)
    wv = ar.alloc([KC, 512], BF16)
    Bwv = p.buf()
    p.dma("pool", wv, w_mem_kv[:, 512:1024].rearrange("(c p) n -> p c n", p=128), writes=[Bwv])
    for mt_ in range(2):
        def mmv(e, mt_=mt_):
            r = None
            for kc in range(KC):
                r = e.matmul(k.ps[mt_][:], lhsT=mhT[:, kc, mt_ * 128:(mt_ + 1) * 128], rhs=wv[:, kc, :],
                             start=(kc == 0), stop=(kc == KC - 1))
            return r
        p.op("pe", mmv, reads=[Bmh, Bwv], writes=[k.Bps[mt_]])
        k.copy(k.ev_eng(), vm[:, mt_, :], k.ps[mt_][:], [k.Bps[mt_]], [Bvm])
    qmT = ar.alloc([T], BF16)
    BqmT = p.buf("qmT")
    pT_ = [ar.alloc([2, 512], BF16) for _ in range(2)]
    BpT_ = p.bufs(2, "pTm")
    rs = ar.alloc([512], F32)
    Brs = p.buf()
    mo_s = ar.alloc([T], BF16)
    Bmo = p.buf("mo_s")
    wqb = ([ar.alloc([KC, 128], BF16) for _ in range(2)], p.bufs(2, "wq"))
    for h in range(4):
        def ev_qm(ci, t0, nt, ps_ap, Bp):
            k.qknorm(ps_ap, Bp, nt, gq_m, Bgqm, qmT[:, t0:t0 + nt], BqmT, scr)
        k.proj_F(lambda c0, n, h=h: w_in[:, 3584 + h * 128 + c0:3584 + h * 128 + c0 + n], 128, hT, Bh, 256, T,
                 ev_qm, wblk=128, psb=(0, 1), wbufs=wqb)
        for tt in range(4):
            b = tt % 2
            for m2 in range(2):
                pi = 2 + m2
                p.op("pe", lambda e, m2=m2, tt=tt, h=h, pi=pi: e.matmul(
                    k.ps[pi][:], lhsT=kTm[:, h, m2 * 128:(m2 + 1) * 128], rhs=qmT[:, tt * 512:(tt + 1) * 512],
                    start=True, stop=True), reads=[BkTm, BqmT], writes=[k.Bps[pi]])
                p.op("act", lambda e, m2=m2, b=b, pi=pi: e.activation(out=pT_[b][:, m2, :], in_=k.ps[pi][:], func=AF.Exp),
                     reads=[k.Bps[pi]], writes=[BpT_[b]])

            def mo_mm(e, b=b, h=h):
                r = None
                for m2 in range(2):
                    r = e.matmul(k.ps[5][:], lhsT=vm[:, m2, h * 128:(h + 1) * 128], rhs=pT_[b][:, m2, :],
                                 start=(m2 == 0), stop=(m2 == 1))
                return r
            p.op("pe", mo_mm, reads=[Bvm, BpT_[b]], writes=[k.Bps[5]])

            def sm_mm(e, b=b):
                r = None
                for m2 in range(2):
                    r = e.matmul(k.ps[6][:], lhsT=k.ones, rhs=pT_[b][:, m2, :], start=(m2 == 0), stop=(m2 == 1))
                return r
            p.op("pe", sm_mm, reads=[k.Bc, BpT_[b]], writes=[k.Bps[6]])
            p.op("dve", lambda e: e.reciprocal(out=rs, in_=k.ps[6][:]), reads=[k.Bps[6]], writes=[Brs])
            p.op("dve", lambda e, tt=tt: e.tensor_tensor(out=mo_s[:, tt * 512:(tt + 1) * 512], in0=k.ps[5][:], in1=rs,
                                                         op=ALU.mult), reads=[k.Bps[5], Brs], writes=[Bmo])
        p.dma("sp", mo_d[h], mo_s, reads=[Bmo])

    k.phase()
    ar.off = mark_h
    gq_n, Bgqn = k.load_gvec(na_q_g, SC)
    gk_n, Bgkn = k.load_gvec(na_k_g, 1.0)
    rm = ar.alloc([5, 768], F32)
    Brm = p.buf("rowmask")
    p.dma("sp", rm, rowmask.rearrange("v k j q -> k v (j q)"), writes=[Brm])
    bias = ar.alloc([5, 768], F32)
    Bbias = p.buf("bias")
    p.op("pool", lambda e: e.memset(bias, 0.0), writes=[Bbias])
    scr = k.qk_scratch(4)
    qT = ar.alloc([T], BF16)
    BqT = p.buf("qT")
    kT = ar.alloc([TE], BF16)
    BkT = p.buf("kT")
    vh = ar.alloc([20, 128], BF16)
    Bvh = p.buf("vh")
    wvh = ar.alloc([KC, 128], BF16)
    Bwvh = p.buf("wvh")
    e1 = [ar.alloc([6, 128], F32) for _ in range(2)]
    Be1 = p.bufs(2, "e1")
    pt = [ar.alloc([6, 128], BF16) for _ in range(2)]
    Bpt = p.bufs(2, "pt")
    rsn = ar.alloc([128], F32)
    Brsn = p.buf()
    na_s = ar.alloc([T], BF16)
    Bna = p.buf("na_s")
    wqb = ([ar.alloc([KC, 128], BF16) for _ in range(2)], p.bufs(2, "wqn"))

    def variant(m):
        return {0: 0, 1: 1, 14: 3, 15: 4}.get(m, 2)

    def tiles_of(m):
        if m == 0:
            return list(range(0, 6))
        if m == 15:
            return list(range(14, 20))
        return list(range(m, m + 5))
    rep_m = {0: 0, 1: 1, 2: 2, 3: 14, 4: 15}
    for h in range(8):
        fresh = []
        p.op("pool", lambda e: e.nop(), writes=[Bbias])
        for v in range(5):
            m = rep_m[v]
            for jj, j in enumerate(tiles_of(m)):
                for kr in range(2):
                    dr0 = (2 * j - 4 + kr) - (2 * m)
                    lo, hi = 7 - dr0, 7 - dr0 + 1
                    for qr, ri in ((0, lo), (1, hi)):
                        if 0 <= ri <= 14:
                            fb_ = p.buf()
                            fresh.append(fb_)
                            p.dma("sp", bias[kr * 64:(kr + 1) * 64, v, jj * 128 + qr * 64: jj * 128 + qr * 64 + 64],
                                  ttab[h, ri], reads=[Bbias], writes=[fb_])
        p.op("pool", lambda e: e.tensor_tensor(out=bias, in0=bias, in1=rm, op=ALU.add), reads=[Brm] + fresh, writes=[Bbias])

        def ev_q(ci, t0, nt, ps_ap, Bp):
            k.qknorm(ps_ap, Bp, nt, gq_n, Bgqn, qT[:, t0:t0 + nt], BqT, scr)
        k.proj_F(lambda c0, n, h=h: w_in[:, 512 + h * 128 + c0:512 + h * 128 + c0 + n], 128, hT, Bh, 256, T, ev_q,
                 wblk=128, psb=(0, 1), wbufs=wqb)

        def ev_k(ci, t0, nt, ps_ap, Bp):
            k.qknorm(ps_ap, Bp, nt, gk_n, Bgkn, kT[:, t0:t0 + nt], BkT, scr)
        k.proj_F(lambda c0, n, h=h: w_in[:, 1536 + h * 128 + c0:1536 + h * 128 + c0 + n], 128, hT, Bh, 0, TE, ev_k,
                 wblk=128, psb=(0, 1), wbufs=wqb)
        p.dma("pool", wvh, w_in[:, 2560 + h * 128:2560 + (h + 1) * 128].rearrange("(c p) n -> p c n", p=128),
              writes=[Bwvh])
        for i4 in range(5):
            pi = 2 + (i4 % 2)

            def mmv(e, i4=i4, pi=pi):
                r = None
                for j in range(4):
                    i = i4 * 4 + j
                    for kc in range(KC):
                        r = e.matmul(k.ps[pi][:, j * 128:(j + 1) * 128], lhsT=hT[:, kc, i * 128:(i + 1) * 128],
                                     rhs=wvh[:, kc, :], start=(kc == 0), stop=(kc == KC - 1))
                return r
            p.op("pe", mmv, reads=[Bh, Bwvh], writes=[k.Bps[pi]])
            k.copy(k.ev_eng(), vh[:, i4 * 4:(i4 + 1) * 4, :], k.ps[pi][:].rearrange("p (a b) -> p a b", b=128),
                   [k.Bps[pi]], [Bvh])
        for m in range(16):
            b = m % 2
            v = variant(m)
            tl = tiles_of(m)
            NT = len(tl)
            pa, pb_ = (4, 5) if b == 0 else (6, 7)

            def sc(e, m=m, tl=tl, pa=pa, pb_=pb_):
                r = None
                for jj, j in enumerate(tl):
                    dst = k.ps[pa][:, jj * 128:(jj + 1) * 128] if jj < 4 else k.ps[pb_][:, (jj - 4) * 128:(jj - 3) * 128]
                    r = e.matmul(dst, lhsT=kT[:, j * 128:(j + 1) * 128], rhs=qT[:, m * 128:(m + 1) * 128],
                                 start=True, stop=True)
                return r
            p.op("pe", sc, reads=[BkT, BqT], writes=[k.Bps[pa], k.Bps[pb_]])
            p.op("dve", lambda e, b=b, v=v, pa=pa: e.tensor_tensor(
                out=e1[b][:, 0:4, :], in0=k.ps[pa][:].rearrange("p (a b) -> p a b", b=128),
                in1=bias[:, v, 0:512].rearrange("p (a b) -> p a b", b=128), op=ALU.add),
                reads=[k.Bps[pa], Bbias], writes=[Be1[b]])
            p.op("dve", lambda e, b=b, v=v, pb_=pb_, NT=NT: e.tensor_tensor(
                out=e1[b][:, 4:NT, :], in0=k.ps[pb_][:, 0:(NT - 4) * 128].rearrange("p (a b) -> p a b", b=128),
                in1=bias[:, v, 512:NT * 128].rearrange("p (a b) -> p a b", b=128), op=ALU.add),
                reads=[k.Bps[pb_], Bbias], writes=[Be1[b]])
            p.op("act", lambda e, b=b, NT=NT: e.activation(out=pt[b][:, 0:NT, :], in_=e1[b][:, 0:NT, :], func=AF.Exp),
                 reads=[Be1[b]], writes=[Bpt[b]])
            po = 2 + b

            def ov(e, b=b, tl=tl, po=po):
                r = None
                for jj, j in enumerate(tl):
                    r = e.matmul(k.ps[po][:, 0:128], lhsT=vh[:, j, :], rhs=pt[b][:, jj, :], start=(jj == 0),
                                 stop=(jj == len(tl) - 1))
                for jj, j in enumerate(tl):
                    r = e.matmul(k.ps[po][:, 128:256], lhsT=k.ones, rhs=pt[b][:, jj, :], start=(jj == 0),
                                 stop=(jj == len(tl) - 1))
                return r
            p.op("pe", ov, reads=[Bvh, Bpt[b], k.Bc], writes=[k.Bps[po]])
            p.op("dve", lambda e, po=po: e.reciprocal(out=rsn, in_=k.ps[po][:, 128:256]), reads=[k.Bps[po]], writes=[Brsn])
            p.op("dve", lambda e, po=po, m=m: e.tensor_tensor(out=na_s[:, m * 128:(m + 1) * 128], in0=k.ps[po][:, 0:128],
                                                              in1=rsn, op=ALU.mult),
                 reads=[k.Bps[po], Brsn], writes=[Bna])
        p.dma("sp", na_d[h], na_s, reads=[Bna])

    k.phase()
    w128s = ar.alloc([512], BF16)
    Bw128 = p.buf()
    p.dma("pool", w128s, w128, writes=[Bw128])
    mt = ar.alloc([128, 2, 16], BF16)
    Bmt = p.buf()
    p.dma("pool", mt, mtab, writes=[Bmt])
    zg = ar.alloc([128, 256], BF16)
    Bzg = p.buf("zg")
    ag = ar.alloc([128, 256], BF16)
    Bag = p.buf("ag")
    ftg = [ar.alloc([T], BF16) for _ in range(2)]
    Bftg = p.bufs(2, "ftg")
    fsc = 1.0 / math.sqrt(16384.0 * 128.0)
    for g_ in range(4):
        for r in range(NCORES):
            p.dma("sp", zg[16 * r:16 * (r + 1)], zall[r, g_].rearrange("(t n) c -> t n c", n=128), writes=[Bzg])
        for c2 in range(64):
            pi = c2 % 4

            def s1(e, c2=c2, pi=pi):
                r = None
                for j in range(2):
                    ch = c2 * 2 + j
                    e.matmul(k.ps[pi][:, j * 256:(j + 1) * 256], lhsT=zg[:, :, ch], rhs=w128s[:, 0:256],
                             start=True, stop=False)
                    r = e.matmul(k.ps[pi][:, j * 256:(j + 1) * 256], lhsT=zg[:, :, 128 + ch], rhs=w128s[:, 256:512],
                                 start=False, stop=True)
                return r
            p.op("pe", s1, reads=[Bzg, Bw128], writes=[k.Bps[pi]])
            k.copy(k.ev_eng(), ag[:, c2 * 2:c2 * 2 + 2, :], k.ps[pi][:].rearrange("p (a b) -> p a b", b=256),
                   [k.Bps[pi]], [Bag])
        for q4 in range(4):
            pi = 4 + q4

            def s3(e, q4=q4, pi=pi):
                r = None
                for kk in range(32):
                    k1 = q4 * 32 + kk
                    e.matmul(k.ps[pi][:, kk * 16:(kk + 1) * 16], lhsT=ag[:, :, k1], rhs=mt[:, k1, 0, :],
                             start=True, stop=False)
                    r = e.matmul(k.ps[pi][:, kk * 16:(kk + 1) * 16], lhsT=ag[:, :, 128 + k1], rhs=mt[:, k1, 1, :],
                                 start=False, stop=True)
                return r
            p.op("pe", s3, reads=[Bag, Bmt], writes=[k.Bps[pi]])
            fb = g_ % 2
            outv = ftg[fb].rearrange("p (k2 k1) -> p k2 k1", k1=128)[:, :, q4 * 32:(q4 + 1) * 32]
            inv = k.ps[pi][:].rearrange("p (kk k2) -> p k2 kk", k2=16)
            p.op("act" if q4 % 2 else "dve",
                 (lambda e, outv=outv, inv=inv: e.activation(out=outv, in_=inv, func=AF.Copy, scale=fsc)) if q4 % 2 else
                 (lambda e, outv=outv, inv=inv: e.tensor_scalar(out=outv, in0=inv, scalar1=fsc, scalar2=None, op0=ALU.mult)),
                 reads=[k.Bps[pi]], writes=[Bftg[fb]])
        p.dma("sp", ft_d[g_], ftg[g_ % 2], reads=[Bftg[g_ % 2]])

    k.phase()
    srcT = ar.alloc([KC, 512], BF16)
    Bsrc = p.buf("srcT")
    wc = [ar.alloc([KC, 512], BF16) for _ in range(2)]
    Bwc = p.bufs(2, "wc")
    gt = [ar.alloc([3, 512], F32) for _ in range(2)]
    Bgt = p.bufs(2, "gt")
    t3 = [ar.alloc([512], F32) for _ in range(3)]
    Bt3 = p.bufs(3, "t3")
    mT = ar.alloc([KC, 512], BF16)
    BmT = p.buf("mT")
    wo = [ar.alloc([KC, 512], BF16) for _ in range(2)]
    Bwo = p.bufs(2, "wo")
    xin = [ar.alloc([512], F32) for _ in range(2)]
    Bxin = p.bufs(2, "xin")
    xo = [ar.alloc([512], F32) for _ in range(2)]
    Bxo = p.bufs(2, "xo")
    for tt in range(4):
        tsl = slice(tt * 512, (tt + 1) * 512)
        p.dma("sp", srcT[:, 0:4, :], ft_d[:, :, tsl].rearrange("g p t -> p g t"), writes=[Bsrc])
        p.dma("sp", srcT[:, 4:12, :], na_d[:, :, tsl].rearrange("g p t -> p g t"), writes=[Bsrc])
        p.dma("sp", srcT[:, 12:16, :], mo_d[:, :, tsl].rearrange("g p t -> p g t"), writes=[Bsrc])
        for nb in range(4):
            b = nb % 2
            csl = slice(nb * 512, (nb + 1) * 512)
            p.dma("pool", wc[b][:, 0:4, :], w_f_out[:, csl].rearrange("(c p) n -> p c n", p=128), writes=[Bwc[b]])
            p.dma("pool", wc[b][:, 4:12, :], w_na_out[:, csl].rearrange("(c p) n -> p c n", p=128), writes=[Bwc[b]])
            p.dma("pool", wc[b][:, 12:16, :], w_mem_out[:, csl].rearrange("(c p) n -> p c n", p=128), writes=[Bwc[b]])
            for c in range(4):
                n = nb * 4 + c
                gb = n % 2
                p.dma("sp", gt[gb], gates_d[:, :, tsl].rearrange("(b n) p t -> n p b t", b=3)[n], writes=[Bgt[gb]])
                rng = ((0, 4), (4, 12), (12, 16))
                for br in range(3):
                    def mm(e, br=br, b=b, c=c):
                        r = None
                        lo, hi = rng[br]
                        for kc in range(lo, hi):
                            r = e.matmul(k.ps[br][:], lhsT=wc[b][:, kc, c * 128:(c + 1) * 128], rhs=srcT[:, kc, :],
                                         start=(kc == lo), stop=(kc == hi - 1))
                        return r
                    p.op("pe", mm, reads=[Bwc[b], Bsrc], writes=[k.Bps[br]])
                    p.op("dve", lambda e, br=br, gb=gb: e.tensor_tensor(out=t3[br], in0=k.ps[br][:], in1=gt[gb][:, br, :],
                                                                        op=ALU.mult),
                         reads=[k.Bps[br], Bgt[gb]], writes=[Bt3[br]])
                p.op("pool", lambda e: e.tensor_tensor(out=t3[0], in0=t3[0], in1=t3[1], op=ALU.add),
                     reads=[Bt3[0], Bt3[1]], writes=[Bt3[0]])
                p.op("pool", lambda e, n=n: e.tensor_tensor(out=mT[:, n, :], in0=t3[0], in1=t3[2], op=ALU.add),
                     reads=[Bt3[0], Bt3[2]], writes=[BmT])
        for nb in range(4):
            b = nb % 2
            csl = slice(nb * 512, (nb + 1) * 512)
            p.dma("pool", wo[b], w_o[:, csl].rearrange("(c p) n -> p c n", p=128), writes=[Bwo[b]])
            for s in range(4):
                xb = (nb * 4 + s) % 2
                pi = 4 + xb
                tok0 = tt * 512 + s * 128
                p.dma("sp", xin[xb], x[256 + tok0:256 + tok0 + 128, csl], writes=[Bxin[xb]])

                def mm(e, b=b, s=s, pi=pi):
                    r = None
                    for kc in range(KC):
                        r = e.matmul(k.ps[pi][:], lhsT=mT[:, kc, s * 128:(s + 1) * 128], rhs=wo[b][:, kc, :],
                                     start=(kc == 0), stop=(kc == KC - 1))
                    return r
                p.op("pe", mm, reads=[BmT, Bwo[b]], writes=[k.Bps[pi]])
                p.op("dve", lambda e, xb=xb, pi=pi: e.tensor_tensor(out=xo[xb], in0=k.ps[pi][:], in1=xin[xb], op=ALU.add),
                     reads=[k.Bps[pi], Bxin[xb]], writes=[Bxo[xb]])
                p.dma("sp", xmid_d[tok0:tok0 + 128, csl], xo[xb], reads=[Bxo[xb]])

    k.phase()
    FH = (16, 16, 12)
    h2T = ar.alloc([KC, 512], BF16)
    Bh2 = p.buf("h2T")
    yacc = ar.alloc([4, D], F32)
    By = p.buf("yacc")
    actT = ar.alloc([16, 512], BF16)
    Bact = p.buf("actT")
    wgu = [[ar.alloc([KC, 512], BF16) for _ in range(2)] for _ in range(2)]
    Bwgu = [p.bufs(2, "wg"), p.bufs(2, "wu")]
    wdn = [ar.alloc([16, 256], BF16) for _ in range(2)]
    Bwdn = p.bufs(2, "wdn")
    sl = [ar.alloc([512], F32) for _ in range(2)]
    Bsl = p.bufs(2, "sl")
    if moe:
        wr = ar.alloc([KC, 8], F32)
        Bwr = p.buf("wr")
        p.dma("sp", wr, wr_d.rearrange("(c p) n -> p c n", p=128), writes=[Bwr])
        lg = ar.alloc([8], F32)
        Blg_s = p.buf()
        m8 = ar.alloc([8], F32)
        Bm8 = p.buf()
        sc4 = ar.alloc([4], F32)
        Bsc4 = p.buf()
        msk = [ar.alloc([8], F32) for _ in range(2)]
        Bmsk = p.bufs(2, "msk")
        wts = ar.alloc([4, 8], F32)
        Bwts = p.buf("wts")
    mark_f = ar.off
    outs = []
    for qt in range(4):
        if qt:
            p.barrier()
        ar.off = mark_f

        def keep(n, xt_ap, Bxt):
            p.op("pool", lambda e: e.tensor_copy(out=yacc[:, n, :], in_=xt_ap), reads=[Bxt], writes=[By])
        router = None
        if moe:
            def done(n):
                p.op("dve", lambda e: e.tensor_copy(out=lg, in_=k.ps[4][:, 0:8]), reads=[k.Bps[4]], writes=[Blg_s])
                p.op("dve", lambda e: e.max(out=m8, in_=lg), reads=[Blg_s], writes=[Bm8])
                p.op("dve", lambda e: e.tensor_scalar(out=sc4[:, 0:1], in0=m8[:, 0:1], scalar1=-1.0, scalar2=None, op0=ALU.mult),
                     reads=[Bm8], writes=[Bsc4])
                p.op("act", lambda e: e.activation(out=sc4[:, 1:2], in_=m8[:, 1:2], func=AF.Exp, bias=sc4[:, 0:1], scale=1.0),
                     reads=[Bm8, Bsc4], writes=[Bsc4])
                p.op("dve", lambda e: e.tensor_scalar(out=sc4[:, 2:3], in0=sc4[:, 1:2], scalar1=1.0, scalar2=None, op0=ALU.add),
                     reads=[Bsc4], writes=[Bsc4])
                p.op("dve", lambda e: e.reciprocal(out=sc4[:, 2:3], in_=sc4[:, 2:3]), reads=[Bsc4], writes=[Bsc4])
                p.op("dve", lambda e: e.tensor_tensor(out=sc4[:, 3:4], in0=sc4[:, 1:2], in1=sc4[:, 2:3], op=ALU.mult),
                     reads=[Bsc4], writes=[Bsc4])
                p.op("dve", lambda e: e.tensor_scalar(out=msk[0], in0=lg, scalar1=m8[:, 0:1], scalar2=None, op0=ALU.is_equal),
                     reads=[Blg_s, Bm8], writes=[Bmsk[0]])
                p.op("dve", lambda e: e.tensor_scalar(out=msk[1], in0=lg, scalar1=m8[:, 1:2], scalar2=None, op0=ALU.is_equal),
                     reads=[Blg_s, Bm8], writes=[Bmsk[1]])
                p.op("dve", lambda e: e.tensor_scalar(out=wts[:, n, :], in0=msk[0], scalar1=sc4[:, 2:3], scalar2=None, op0=ALU.mult),
                     reads=[Bmsk[0], Bsc4], writes=[Bwts])
                p.op("dve", lambda e: e.scalar_tensor_tensor(out=wts[:, n, :], in0=msk[1], scalar=sc4[:, 3:4], in1=wts[:, n, :],
                                                             op0=ALU.mult, op1=ALU.add),
                     reads=[Bmsk[1], Bsc4, Bwts], writes=[Bwts])
            router = (wr, Bwr, k.ps[4][:, 0:8], k.Bps[4], done)
        k.norm_tiles(lambda i: xmid_d[i * 128:(i + 1) * 128, :], ln_ffn_g, h2T,
                     [(qt * 4 + n, n * 128) for n in range(4)], Bh2, keep=keep, router=router)
        for ex in range(NE):
            ff0 = 0
            for half in range(len(FH)):
                nch = FH[half]
                for blk in range(nch // 4):
                    b = blk % 2
                    c0 = (ff0 + blk * 4) * 128
                    p.dma("pool", wgu[0][b], wg_d[ex][:, c0:c0 + 512].rearrange("(c p) n -> p c n", p=128), writes=[Bwgu[0][b]])
                    p.dma("pool", wgu[1][b], wu_d[ex][:, c0:c0 + 512].rearrange("(c p) n -> p c n", p=128), writes=[Bwgu[1][b]])
                    for c in range(4):
                        sb_ = (blk * 4 + c) % 2
                        pg, pu = (0, 1) if sb_ == 0 else (2, 3)
                        for gu, pi in ((0, pg), (1, pu)):
                            def mm(e, gu=gu, pi=pi, b=b, c=c):
                                r = None
                                for kc in range(KC):
                                    r = e.matmul(k.ps[pi][:], lhsT=wgu[gu][b][:, kc, c * 128:(c + 1) * 128], rhs=h2T[:, kc, :],
                                                 start=(kc == 0), stop=(kc == KC - 1))
                                return r
                            p.op("pe", mm, reads=[Bwgu[gu][b], Bh2], writes=[k.Bps[pi]])
                        p.op("act", lambda e, sb_=sb_, pg=pg: e.activation(out=sl[sb_], in_=k.ps[pg][:], func=AF.Silu),
                             reads=[k.Bps[pg]], writes=[Bsl[sb_]])
                        p.op("dve", lambda e, sb_=sb_, pu=pu, ci=blk * 4 + c: e.tensor_tensor(
                            out=actT[:, ci, :], in0=k.ps[pu][:], in1=sl[sb_], op=ALU.mult),
                            reads=[k.Bps[pu], Bsl[sb_]], writes=[Bact])
                for nb in range(8):
                    b = nb % 2
                    p.dma("pool", wdn[b][:, 0:nch, :],
                          wd_d[ex][ff0 * 128:(ff0 + nch) * 128, nb * 256:(nb + 1) * 256].rearrange("(c p) n -> p c n", p=128),
                          writes=[Bwdn[b]])
                    for s in range(4):
                        pi = 4 + (nb * 4 + s) % 2 if not moe else 5 + (nb * 4 + s) % 2
                        pi = 5 + (nb * 4 + s) % 2

                        def mm(e, b=b, s=s, pi=pi, nch=nch):
                            r = None
                            for ci in range(nch):
                                r = e.matmul(k.ps[pi][:, 0:256], lhsT=actT[:, ci, s * 128:(s + 1) * 128], rhs=wdn[b][:, ci, :],
                                             start=(ci == 0), stop=(ci == nch - 1))
                            return r
                        p.op("pe", mm, reads=[Bact, Bwdn[b]], writes=[k.Bps[pi]])
                        ysl = yacc[:, s, nb * 256:(nb + 1) * 256]
                        if moe:
                            p.op("dve", lambda e, pi=pi, ysl=ysl, s=s, ex=ex: e.scalar_tensor_tensor(
                                out=ysl, in0=k.ps[pi][:, 0:256], scalar=wts[:, s, ex:ex + 1], in1=ysl, op0=ALU.mult, op1=ALU.add),
                                reads=[k.Bps[pi], Bwts, By], writes=[By])
                        else:
                            p.op("dve", lambda e, pi=pi, ysl=ysl: e.tensor_tensor(out=ysl, in0=k.ps[pi][:, 0:256], in1=ysl, op=ALU.add),
                                 reads=[k.Bps[pi], By], writes=[By])
                ff0 += nch
        for s in range(4):
            tok0 = qt * 512 + s * 128
            outs.extend(out_dma(tok0, yacc[:, s, :], By))
    return outs


def _dft_consts():
    c = np.arange(128)
    ang = 2.0 * np.pi * np.outer(c, c) / 128.0
    C = np.cos(ang)
    S = np.sin(ang)
    cs = np.concatenate([C, -S], axis=1).astype(np.float32)
    w128 = np.concatenate([C, S, S, -C], axis=1).astype(np.float32)
    mt = []
    n2 = np.arange(128)[:, None, None]
    k1 = np.arange(128)[None, :, None]
    for core in range(NCORES):
        k2 = (np.arange(16) + 16 * core)[None, None, :]
        kk = (k1 + 128 * k2)
        a = 2.0 * np.pi * ((n2 * kk) % 16384) / 16384.0
        mt.append(np.stack([np.cos(a), -np.sin(a)], axis=2).astype(np.float32))
    return cs, w128, mt


def _na_tables(rpb):
    cols = np.arange(64)
    cstart = np.clip(cols - 8, 0, 48)
    kc = np.arange(64)[:, None]
    qc = np.arange(64)[None, :]
    valid = (kc >= cstart[None, :]) & (kc < cstart[None, :] + 16)
    off = np.clip(kc - qc + 15, 0, 30)
    tt = np.full((8, 16, 64, 64), NEG, np.float32)
    for ri in range(15):
        dr = 7 - ri
        g = rpb[:, dr + 7][:, off]
        tt[:, ri] = np.where(valid[None], g, np.float32(NEG))
    return tt


def _rowmask(core):
    rm = np.full((5, 128, 6, 128), NEG, np.float32)
    rep_m = {0: 0, 1: 1, 2: 2, 3: 14, 4: 15}
    for v in range(5):
        m = rep_m[v]
        tl = list(range(0, 6)) if m == 0 else (list(range(14, 20)) if m == 15 else list(range(m, m + 5)))
        for jj, j in enumerate(tl):
            for kr in range(2):
                krow = 32 * core + (2 * j - 4 + kr)
                for qr in range(2):
                    qrow = 32 * core + 2 * m + qr
                    rs = min(max(qrow - 4, 0), 256 - 8)
                    if rs <= krow < rs + 8:
                        rm[v, kr * 64:(kr + 1) * 64, jj, qr * 64:(qr + 1) * 64] = 0.0
    return rm


_CACHE = {}
_DBG_NE = 0
_DBG_L = 2
FUSED = False


def build_fused():
    nc = bass.Bass("TRN2", target_bir_lowering=False)
    I32 = mybir.dt.int32
    din = lambda name, shape, dt=F32: nc.dram_tensor(name, shape, dt, kind="ExternalInput").ap()
    x0 = din("x", [TE, D])
    mem = din("mem", [256, D])
    ln_mix_g = din("ln_mix_g", [2, D])
    w_in = din("w_in", [2, D, 10240])
    na_q_g = din("na_q_g", [2, 128])
    na_k_g = din("na_k_g", [2, 128])
    ttab = din("ttab", [2, 8, 16, 64, 64])
    rowmask = din("rowmask", [5, 128, 6, 128])
    mem_ln_g = din("mem_ln_g", [2, D])
    w_mem_kv = din("w_mem_kv", [2, D, 1024])
    mem_q_g = din("mem_q_g", [2, 128])
    mem_k_g = din("mem_k_g", [2, 128])
    w_f_out = din("w_fourier_out", [2, 512, D])
    w_na_out = din("w_na_out", [2, 1024, D])
    w_mem_out = din("w_mem_out", [2, 512, D])
    w_o = din("w_o", [2, D, D])
    ln_ffn_g = din("ln_ffn_g", [2, D])
    cs = din("cs", [128, 256])
    w128 = din("w128", [128, 512])
    mtab = din("mtab", [128, 128, 2, 16])
    hidx = din("hidx", [128, 4], I32)
    ffn_wg = din("ffn_w_gate", [1, D, DFF])
    ffn_wu = din("ffn_w_up", [1, D, DFF])
    ffn_wd = din("ffn_w_down", [1, DFF, D])
    wr_d = din("moe_router", [D, 8])
    moe_wg = din("moe_w_gate", [8, D, DFF])
    moe_wu = din("moe_w_up", [8, D, DFF])
    moe_wd = din("moe_w_down", [8, DFF, D])
    xout = nc.dram_tensor("xout", [T, D], F32, kind="ExternalOutput").ap()
    dsc = lambda name, shape, dt, **kw: nc.dram_tensor(name, shape, dt, **kw)
    scratch = {"ft_d": dsc("ft_d", [4, 128, T], BF16).ap(), "na_d": dsc("na_d", [8, 128, T], BF16).ap(),
               "mo_d": dsc("mo_d", [4, 128, T], BF16).ap(), "gates_d": dsc("gates_d", [48, 128, T], F32).ap(),
               "xmid_d": dsc("xmid_d", [T, D], F32).ap()}
    z_own = [dsc(f"z_own{l}", [4 * T, 256], BF16) for l in range(2)]
    z_all = [dsc(f"z_all{l}", [NCORES * 4 * T, 256], BF16, addr_space="Shared") for l in range(2)]
    x1 = dsc("x1_ext", [TE, D], F32).ap()
    edge_own = dsc("edge_own", [512, D], F32)
    edge_all = dsc("edge_all", [NCORES * 512, D], F32, addr_space="Shared")
    with ExitStack() as st:
        k = K(nc, st)
        p, ar = k.p, k.ar
        k.consts()
        final = []
        for l in range(_DBG_L):
            xl = x0 if l == 0 else x1
            if l == 1:
                k.phase()
                Bcc = p.buf("edge_all")
                p.cc("AllGather", [edge_own.ap().opt()], [edge_all.ap().opt()], writes=[Bcc])
                idx = ar.alloc([4], I32)
                Bidx = p.buf()
                p.dma("sp", idx, hidx, writes=[Bidx])
                ht = [ar.alloc([D], F32) for _ in range(2)]
                Bht = p.bufs(2, "ht")
                for j in range(4):
                    b = j % 2
                    def gat(e, j=j, b=b):
                        return e.indirect_dma_start(out=ht[b], out_offset=None, in_=edge_all.ap(),
                                                    in_offset=bass.IndirectOffsetOnAxis(ap=idx[:, j:j + 1], axis=0))
                    p.op("pool", gat, reads=[Bcc, Bidx], writes=[Bht[b]], is_dma=True)
                    row0 = j * 128 if j < 2 else 2304 + (j - 2) * 128
                    p.dma("sp", x1[row0:row0 + 128, :], ht[b], reads=[Bht[b]])
            k.phase()
            zo = z_own[l].ap().rearrange("(g t) c -> g t c", g=4)
            phase_fin(k, lambda i, xl=xl: xl[256 + i * 128:256 + (i + 1) * 128, :], ln_mix_g[l],
                      lambda c0, n, l=l: w_in[l][:, c0:c0 + n], cs, zo, own_off=0, do_norm=True, hT=None, Bh=None)
            k.phase()
            Bza = p.buf("zall")
            p.cc("AllGather", [z_own[l].ap().opt()], [z_all[l].ap().opt()], writes=[Bza])
            zall = z_all[l].ap().rearrange("(r g t) c -> r g t c", r=NCORES, g=4)
            W = {"mem": mem, "ln_mix_g": ln_mix_g[l], "w_in": w_in[l], "na_q_g": na_q_g[l], "na_k_g": na_k_g[l],
                 "ttab": ttab[l], "rowmask": rowmask, "mem_ln_g": mem_ln_g[l], "w_mem_kv": w_mem_kv[l],
                 "mem_q_g": mem_q_g[l], "mem_k_g": mem_k_g[l], "w_f_out": w_f_out[l], "w_na_out": w_na_out[l],
                 "w_mem_out": w_mem_out[l], "w_o": w_o[l], "ln_ffn_g": ln_ffn_g[l], "w128": w128, "mtab": mtab}
            W.update(scratch)
            if l == 0 and _DBG_L == 1:
                W.update({"wg": ffn_wg, "wu": ffn_wu, "wd": ffn_wd})

                def out_dma(tok0, src, Bsrc):
                    return [p.dma("sp", xout[tok0:tok0 + 128, :], src, reads=[Bsrc])]
            elif l == 0:
                W.update({"wg": ffn_wg, "wu": ffn_wu, "wd": ffn_wd})

                def out_dma(tok0, src, Bsrc):
                    ops = [p.dma("sp", x1[256 + tok0:256 + tok0 + 128, :], src, reads=[Bsrc])]
                    if tok0 < 256:
                        ops.append(p.dma("sp", edge_own.ap()[tok0:tok0 + 128, :], src, reads=[Bsrc]))
                    if tok0 >= T - 256:
                        e0 = 256 + tok0 - (T - 256)
                        ops.append(p.dma("sp", edge_own.ap()[e0:e0 + 128, :], src, reads=[Bsrc]))
                    return ops
            else:
                W.update({"wg": moe_wg, "wu": moe_wu, "wd": moe_wd, "wr": wr_d})

                def out_dma(tok0, src, Bsrc):
                    return [p.dma("sp", xout[tok0:tok0 + 128, :], src, reads=[Bsrc])]
            k.phase()
            outs = emit_layer(k, nc, l, xl, zall, W, out_dma)
            if l == _DBG_L - 1:
                final = outs
        p.emit(final_wait_ops=final)
    return nc


def build_unfused_a():
    nc = bass.Bass("TRN2", target_bir_lowering=False)
    x = nc.dram_tensor("x", [T, D], F32, kind="ExternalInput").ap()
    g = nc.dram_tensor("g", [D], F32, kind="ExternalInput").ap()
    wf = nc.dram_tensor("wf", [D, 512], F32, kind="ExternalInput").ap()
    cs = nc.dram_tensor("cs", [128, 256], F32, kind="ExternalInput").ap()
    z = nc.dram_tensor("z", [4, T, 256], BF16, kind="ExternalOutput").ap()
    with ExitStack() as st:
        k = K(nc, st)
        k.consts()
        fin = phase_fin(k, lambda i: x[i * 128:(i + 1) * 128, :], g, lambda c0, n: wf[:, c0:c0 + n], cs, z, own_off=0,
                        do_norm=True, hT=None, Bh=None)
        k.p.emit(final_wait_ops=fin)
    return nc


def build_unfused_b(layer):
    moe = (layer % 2 == 1)
    nc = bass.Bass("TRN2", target_bir_lowering=False)
    din = lambda name, shape, dt=F32: nc.dram_tensor(name, shape, dt, kind="ExternalInput").ap()
    x = din("x", [TE, D])
    zall = din("zall", [NCORES, 4, T, 256], BF16)
    W = {"mem": din("mem", [256, D]), "ln_mix_g": din("ln_mix_g", [D]), "w_in": din("w_in", [D, 10240]),
         "na_q_g": din("na_q_g", [128]), "na_k_g": din("na_k_g", [128]), "ttab": din("ttab", [8, 16, 64, 64]),
         "rowmask": din("rowmask", [5, 128, 6, 128]), "mem_ln_g": din("mem_ln_g", [D]),
         "w_mem_kv": din("w_mem_kv", [D, 1024]), "mem_q_g": din("mem_q_g", [128]), "mem_k_g": din("mem_k_g", [128]),
         "w_f_out": din("w_f_out", [512, D]), "w_na_out": din("w_na_out", [1024, D]), "w_mem_out": din("w_mem_out", [512, D]),
         "w_o": din("w_o", [D, D]), "ln_ffn_g": din("ln_ffn_g", [D]), "w128": din("w128", [128, 512]),
         "mtab": din("mtab", [128, 128, 2, 16])}
    if moe:
        W.update({"wr": din("moe_router", [D, 8]), "wg": din("moe_w_gate", [8, D, DFF]), "wu": din("moe_w_up", [8, D, DFF]),
                  "wd": din("moe_w_down", [8, DFF, D])})
    else:
        W.update({"wg": din("ffn_w_gate", [1, D, DFF]), "wu": din("ffn_w_up", [1, D, DFF]), "wd": din("ffn_w_down", [1, DFF, D])})
    xout = nc.dram_tensor("xout", [T, D], F32, kind="ExternalOutput").ap()
    dsc = lambda name, shape, dt: nc.dram_tensor(name, shape, dt).ap()
    W.update({"ft_d": dsc("ft_d", [4, 128, T], BF16), "na_d": dsc("na_d", [8, 128, T], BF16), "mo_d": dsc("mo_d", [4, 128, T], BF16),
              "gates_d": dsc("gates_d", [48, 128, T], F32), "xmid_d": dsc("xmid_d", [T, D], F32)})
    with ExitStack() as st:
        k = K(nc, st)
        k.consts()
        p = k.p

        def out_dma(tok0, src, Bsrc):
            return [p.dma("sp", xout[tok0:tok0 + 128, :], src, reads=[Bsrc])]
        outs = emit_layer(k, nc, layer, x, zall, W, out_dma)
        p.emit(final_wait_ops=outs)
    return nc


def kernel_unfused(inp):
    x = np.ascontiguousarray(inp["x"][0])
    mem = np.ascontiguousarray(inp["mem"][0])
    cs, w128, mtabs = _dft_consts()
    rms = [_rowmask(c) for c in range(NCORES)]
    cores = list(range(NCORES))
    C = np.ascontiguousarray
    for l in range(2):
        if "a" not in _CACHE:
            _CACHE["a"] = build_unfused_a()
        g = C(inp["ln_mix_g"][l])
        ins = [{"x": C(x[c * T:(c + 1) * T]), "g": g, "wf": C(inp["w_in"][l][:, :512]), "cs": cs} for c in cores]
        res = run_bass_kernel_spmd(_CACHE["a"], ins, core_ids=cores)
        zall = np.stack([np.asarray(r["z"]) for r in res.results], axis=0)
        if ("b", l) not in _CACHE:
            _CACHE[("b", l)] = build_unfused_b(l)
        xp = np.concatenate([np.zeros((256, D), np.float32), x, np.zeros((256, D), np.float32)], axis=0)
        common = {"zall": zall, "mem": mem, "ln_mix_g": g, "w_in": C(inp["w_in"][l]), "na_q_g": C(inp["na_q_g"][l]),
                  "na_k_g": C(inp["na_k_g"][l]), "ttab": _na_tables(inp["na_rpb"][l]), "mem_ln_g": C(inp["mem_ln_g"][l]),
                  "w_mem_kv": C(inp["w_mem_kv"][l]), "mem_q_g": C(inp["mem_q_g"][l]), "mem_k_g": C(inp["mem_k_g"][l]),
                  "w_f_out": C(inp["w_fourier_out"][l]), "w_na_out": C(inp["w_na_out"][l]), "w_mem_out": C(inp["w_mem_out"][l]),
                  "w_o": C(inp["w_o"][l]), "ln_ffn_g": C(inp["ln_ffn_g"][l]), "w128": w128}
        i = l // 2
        if l % 2 == 1:
            common.update({"moe_router": C(inp["moe_router"][i]), "moe_w_gate": inp["moe_w_gate"][i],
                           "moe_w_up": inp["moe_w_up"][i], "moe_w_down": inp["moe_w_down"][i]})
        else:
            common.update({"ffn_w_gate": inp["ffn_w_gate"][i:i + 1], "ffn_w_up": inp["ffn_w_up"][i:i + 1],
                           "ffn_w_down": inp["ffn_w_down"][i:i + 1]})
        ins = []
        for c in cores:
            d = dict(common)
            d["x"] = C(xp[c * T:c * T + TE])
            d["rowmask"] = rms[c]
            d["mtab"] = mtabs[c]
            ins.append(d)
        res = run_bass_kernel_spmd(_CACHE[("b", l)], ins, core_ids=cores)
        x = np.concatenate([np.asarray(r["xout"]) for r in res.results], axis=0)
    return x.reshape(1, NCORES * T, D).astype(np.float32)


def kernel(**inp):
    inp = {k_: np.asarray(v) for k_, v in inp.items()}
    if not FUSED:
        return kernel_unfused(inp)
    x = inp["x"][0]
    cs, w128, mtabs = _dft_consts()
    cores = list(range(NCORES))
    if "nc" not in _CACHE:
        _CACHE["nc"] = build_fused()
    nc = _CACHE["nc"]
    xp = np.concatenate([np.zeros((256, D), np.float32), x, np.zeros((256, D), np.float32)], axis=0)
    ttab = np.stack([_na_tables(inp["na_rpb"][l]) for l in range(2)], axis=0)
    common = {k_: inp[k_] for k_ in ("ln_mix_g", "w_in", "na_q_g", "na_k_g", "mem_ln_g", "w_mem_kv", "mem_q_g", "mem_k_g",
                                     "w_fourier_out", "w_na_out", "w_mem_out", "w_o", "ln_ffn_g", "ffn_w_gate", "ffn_w_up",
                                     "ffn_w_down")}
    common.update({"mem": inp["mem"][0], "ttab": ttab, "cs": cs, "w128": w128, "moe_router": inp["moe_router"][0],
                   "moe_w_gate": inp["moe_w_gate"][0], "moe_w_up": inp["moe_w_up"][0], "moe_w_down": inp["moe_w_down"][0]})
    ins = []
    r = np.arange(128)
    for c in cores:
        d = dict(common)
        d["x"] = np.ascontiguousarray(xp[c * T:c * T + TE])
        d["rowmask"] = _rowmask(c)
        d["mtab"] = mtabs[c]
        up = c - 1 if c > 0 else c
        dn = c + 1 if c < NCORES - 1 else c
        hidx = np.stack([up * 512 + 256 + r, up * 512 + 384 + r, dn * 512 + r, dn * 512 + 128 + r], axis=1)
        d["hidx"] = hidx.astype(np.int32)
        ins.append(d)
    res = run_bass_kernel_spmd(nc, ins, core_ids=cores)
    out = np.concatenate([np.asarray(r_["xout"]) for r_ in res.results], axis=0)
    return out.reshape(1, NCORES * T, D).astype(np.float32)
```

```python
import math
from contextlib import ExitStack
import numpy as np
import ml_dtypes
import concourse.bass as bass
import concourse.mybir as mybir
from concourse.bass_utils import run_bass_kernel_spmd

F32 = mybir.dt.float32
BF16 = mybir.dt.bfloat16
ALU = mybir.AluOpType
AF = mybir.ActivationFunctionType

NCORES = 8
D = 2048
KC = 16
T = 2048
TE = 2560
DFF = 5632
NFF = 44
NEG = -30000.0
EPS = 1e-6


class Buf:
    __slots__ = ("name", "last_w", "readers")

    def __init__(self, name):
        self.name = name
        self.last_w = None
        self.readers = []


class Op:
    __slots__ = ("eng", "fn", "waits", "needs_inc", "is_dma", "sem", "sem_val", "milestone")

    def __init__(self, eng, fn, is_dma=False):
        self.eng = eng
        self.fn = fn
        self.waits = []
        self.needs_inc = False
        self.is_dma = is_dma
        self.sem = None
        self.sem_val = 0
        self.milestone = 0


ENGS = ("pe", "dve", "act", "pool", "sp")


class Prog:
    def __init__(self, nc, n_dma_sems=32):
        self.nc = nc
        self.q = {e: [] for e in ENGS}
        self.n_dma_sems = n_dma_sems
        self.dma_count = 0
        self.dma_last = [None] * n_dma_sems
        self.dma_cnt_per = [0] * n_dma_sems
        self.nbuf = 0
        self.cc_ops = []

    def buf(self, name=None):
        self.nbuf += 1
        return Buf(name or f"b{self.nbuf}")

    def bufs(self, n, name="b"):
        return [self.buf(f"{name}{i}") for i in range(n)]

    def _dep(self, op, d):
        if d is None or d is op:
            return
        if (not d.is_dma) and d.eng == "pe" and op.eng == "pe" and not op.is_dma:
            return
        if not d.is_dma:
            d.needs_inc = True
        op.waits.append(d)

    def op(self, eng, fn, reads=(), writes=(), is_dma=False):
        o = Op(eng, fn, is_dma)
        for r in reads:
            self._dep(o, r.last_w)
        for w in writes:
            self._dep(o, w.last_w)
            for rd in w.readers:
                self._dep(o, rd)
        for w in writes:
            w.last_w = o
            w.readers = []
        for r in reads:
            r.readers.append(o)
        if is_dma:
            s = self.dma_count % self.n_dma_sems
            self.dma_count += 1
            prev = self.dma_last[s]
            if prev is not None:
                o.waits.append(prev)
            self.dma_cnt_per[s] += 1
            o.sem = s
            o.sem_val = 16 * self.dma_cnt_per[s]
            self.dma_last[s] = o
        self.q[eng].append(o)
        return o

    def dma(self, eng, out, in_, reads=(), writes=(), **kw):
        def fn(e):
            return e.dma_start(out=out, in_=in_, **kw)
        return self.op(eng, fn, reads, writes, is_dma=True)

    def cc(self, kind, ins, outs, reads=(), writes=()):
        def fn(e):
            return e.collective_compute(kind, ALU.bypass, replica_groups=[list(range(NCORES))], ins=ins, outs=outs)
        o = Op("pool", fn, True)
        for r in reads:
            self._dep(o, r.last_w)
        for w in writes:
            self._dep(o, w.last_w)
            for rd in w.readers:
                self._dep(o, rd)
        for w in writes:
            w.last_w = o
            w.readers = []
        for r in reads:
            r.readers.append(o)
        self.cc_ops.append(o)
        o.sem = -len(self.cc_ops)
        o.sem_val = 1
        self.q["pool"].append(o)
        return o

    def barrier(self):
        lasts = []
        for e in ENGS:
            for o in reversed(self.q[e]):
                if not o.is_dma:
                    lasts.append(o)
                    break
        dl = [d for d in self.dma_last if d is not None] + list(self.cc_ops)
        for e in ENGS:
            o = Op(e, lambda eng: eng.nop())
            for d in lasts:
                if d.eng == e:
                    continue
                d.needs_inc = True
                o.waits.append(d)
            for d in dl:
                o.waits.append(d)
            self.q[e].append(o)

    def emit(self, final_wait_ops=()):
        nc = self.nc
        for e in ENGS:
            c = 0
            for o in self.q[e]:
                if o.is_dma:
                    continue
                if o.needs_inc:
                    c += 1
                    o.milestone = c
        with ExitStack() as st:
            esem = {e: st.enter_context(nc.semaphore(f"s_{e}")) for e in ENGS}
            dsem = [st.enter_context(nc.semaphore(f"d_{i}")) for i in range(self.n_dma_sems)]
            csem = [st.enter_context(nc.semaphore(f"c_{i}")) for i in range(len(self.cc_ops))]
            block = st.enter_context(nc.Block())
            prog = self

            def run(engname, eng):
                waited = {}
                for o in prog.q[engname]:
                    for d in o.waits:
                        if d.is_dma:
                            key = ("d", d.sem)
                            val = d.sem_val
                            sem = dsem[d.sem] if d.sem >= 0 else csem[-d.sem - 1]
                        else:
                            key = ("e", d.eng)
                            val = d.milestone
                            sem = esem[d.eng]
                        if waited.get(key, 0) >= val:
                            continue
                        waited[key] = val
                        eng.wait_ge(sem, val)
                    ins = o.fn(eng)
                    if o.is_dma and o.sem < 0:
                        ins.then_inc(csem[-o.sem - 1])
                    elif o.is_dma:
                        ins.then_inc(dsem[o.sem], 16)
                    elif o.needs_inc:
                        ins.then_inc(esem[engname], 1)
                if engname == "sp":
                    for d in final_wait_ops:
                        eng.wait_ge(dsem[d.sem], d.sem_val)

            @block.tensor
            def _(eng):
                run("pe", eng)

            @block.vector
            def _(eng):
                run("dve", eng)

            @block.scalar
            def _(eng):
                run("act", eng)

            @block.gpsimd
            def _(eng):
                run("pool", eng)

            @block.sync
            def _(eng):
                run("sp", eng)


class Arena:
    def __init__(self, ap32, n32):
        self.ap = ap32
        self.n = n32
        self.off = 0

    def reset(self):
        self.off = 0

    def alloc(self, shape, dt):
        ne = 1
        for s in shape:
            ne *= s
        sz = 2 if dt == BF16 else 4
        n32 = (ne * sz + 3) // 4
        n32 = (n32 + 7) // 8 * 8
        assert self.off + n32 <= self.n, f"arena overflow {self.off}+{n32}>{self.n}"
        a = self.ap[:, self.off:self.off + n32]
        self.off += n32
        if dt != F32:
            a = a.bitcast(dt)
        a = a[:, :ne]
        if len(shape) == 2:
            a = a.rearrange("p (a b) -> p a b", b=shape[1])
        elif len(shape) == 3:
            a = a.rearrange("p (a b c) -> p a b c", b=shape[1], c=shape[2])
        return a


class K:
    def __init__(self, nc, st):
        self.nc = nc
        self.p = Prog(nc)
        N32 = 49152
        big = st.enter_context(nc.sbuf_tensor("arena", [128, N32], F32))
        self.ar = Arena(big, N32)
        cst = st.enter_context(nc.sbuf_tensor("consts", [128, 1024], F32))
        self.car = Arena(cst, 1024)
        self.ps = [st.enter_context(nc.psum_tensor(f"ps{i}", [128, 512], F32)) for i in range(8)]
        self.Bps = self.p.bufs(8, "ps")
        self.Bc = self.p.buf("consts")
        self.rr = 0

    def phase(self):
        self.p.barrier()
        self.ar.reset()

    def ev_eng(self):
        self.rr += 1
        return "act" if self.rr % 2 else "dve"

    def copy(self, eng, out, in_, reads, writes):
        if eng == "act":
            return self.p.op("act", lambda e: e.copy(out=out, in_=in_), reads, writes)
        return self.p.op(eng, lambda e: e.tensor_copy(out=out, in_=in_), reads, writes)

    def consts(self):
        p = self.p
        c = self.car
        self.identf = c.alloc([128], F32)
        self.ident = c.alloc([128], BF16)
        self.ones = c.alloc([128], BF16)
        identf, ident, ones = self.identf, self.ident, self.ones

        def mk(e):
            e.memset(identf, 0.0)
            return e.affine_select(out=identf, in_=identf, pattern=[[-1, 128]], compare_op=ALU.not_equal,
                                   fill=1.0, base=0, channel_multiplier=1)
        p.op("pool", mk, writes=[self.Bc])
        p.op("dve", lambda e: e.tensor_copy(out=ident, in_=identf), reads=[self.Bc], writes=[self.Bc])
        p.op("dve", lambda e: e.memset(ones, 1.0), writes=[self.Bc])
        self.epsc = c.alloc([1], F32)
        epsc = self.epsc
        p.op("dve", lambda e: e.memset(epsc, EPS), writes=[self.Bc])

    def norm_tiles(self, src_rows, g_row, hT, tiles, Bh, keep=None, router=None, lean=False):
        p, ar = self.p, self.ar
        gbc = ar.alloc([D], F32)
        Bg = p.buf()
        p.dma("sp", gbc, g_row.partition_broadcast(128), writes=[Bg])
        if lean:
            xt0 = ar.alloc([D], F32)
            xt = [xt0, xt0]
            Bx0 = p.buf("xt")
            Bx = [Bx0, Bx0]
        else:
            xt = [ar.alloc([D], F32) for _ in range(2)]
            Bx = p.bufs(2, "xt")
        hb = [ar.alloc([D], BF16) for _ in range(2)]
        Bhb = p.bufs(2, "hb")
        if lean:
            junk = None
        else:
            junk = ar.alloc([D], BF16)
            Bj = p.buf()
        ss = [ar.alloc([1], F32) for _ in range(2)]
        Bss = p.bufs(2, "ss")
        if router is not None:
            h2f = None
            Bh2f = None
            hTf = ar.alloc([4, 128], F32)
            BhTf = p.buf()
        pT = [self.ps[6][:].bitcast(BF16), self.ps[7][:].bitcast(BF16)]
        BpT = [self.Bps[6], self.Bps[7]]
        for n, (i, dcol) in enumerate(tiles):
            b = n % 2
            p.dma("sp", xt[b], src_rows(i), writes=[Bx[b]])
            if keep is not None:
                keep(n, xt[b], Bx[b])
            if lean:
                p.op("act", lambda e, b=b: e.activation(out=hb[b], in_=xt[b], func=AF.Square, accum_out=ss[b]),
                     reads=[Bx[b]], writes=[Bhb[b], Bss[b]])
            else:
                p.op("act", lambda e, b=b: e.activation(out=junk, in_=xt[b], func=AF.Square, accum_out=ss[b]),
                     reads=[Bx[b]], writes=[Bj, Bss[b]])
            p.op("act", lambda e, b=b: e.activation(out=ss[b], in_=ss[b], func=AF.Ln, bias=self.epsc, scale=1.0 / D),
                 reads=[Bss[b], self.Bc], writes=[Bss[b]])
            p.op("act", lambda e, b=b: e.activation(out=ss[b], in_=ss[b], func=AF.Exp, scale=-0.5),
                 reads=[Bss[b]], writes=[Bss[b]])
            if router is None:
                p.op("dve", lambda e, b=b: e.scalar_tensor_tensor(out=hb[b], in0=xt[b], scalar=ss[b], in1=gbc,
                                                                  op0=ALU.mult, op1=ALU.mult),
                     reads=[Bx[b], Bss[b], Bg], writes=[Bhb[b]])
            else:
                h2f = xt[b]
                Bh2f = Bx[b]
                p.op("dve", lambda e, b=b: e.scalar_tensor_tensor(out=xt[b], in0=xt[b], scalar=ss[b], in1=gbc,
                                                                  op0=ALU.mult, op1=ALU.mult),
                     reads=[Bss[b], Bg], writes=[Bx[b]])
                p.op("act", lambda e, b=b: e.copy(out=hb[b], in_=xt[b]), reads=[Bx[b]], writes=[Bhb[b]])
                wr, Bwr, lg_ps, Blg, done = router
                for g4 in range(4):
                    def trf(e, g4=g4, h2f=h2f):
                        r = None
                        for j in range(4):
                            kc = g4 * 4 + j
                            r = e.transpose(self.ps[5][:, j * 128:(j + 1) * 128], h2f[:, kc * 128:(kc + 1) * 128],
                                            self.identf)
                        return r
                    p.op("pe", trf, reads=[Bh2f, self.Bc], writes=[self.Bps[5]])
                    p.op("dve", lambda e: e.tensor_copy(out=hTf, in_=self.ps[5][:].rearrange("p (a b) -> p a b", b=128)),
                         reads=[self.Bps[5]], writes=[BhTf])

                    def lgm(e, g4=g4):
                        r = None
                        for j in range(4):
                            kc = g4 * 4 + j
                            r = e.matmul(lg_ps, lhsT=hTf[:, j, :], rhs=wr[:, kc, :], start=(kc == 0), stop=(kc == 15))
                        return r
                    p.op("pe", lgm, reads=[BhTf, Bwr], writes=[Blg])
                done(n)
            for half in range(2):
                def tr(e, b=b, half=half):
                    r = None
                    for j in range(8):
                        kc = half * 8 + j
                        r = e.transpose(pT[half][:, j * 128:(j + 1) * 128], hb[b][:, kc * 128:(kc + 1) * 128], self.ident)
                    return r
                p.op("pe", tr, reads=[Bhb[b], self.Bc], writes=[BpT[half]])
                self.copy("act" if half else "dve", hT[:, half * 8:(half + 1) * 8, dcol:dcol + 128],
                          pT[half].rearrange("p (a b) -> p a b", b=128), [BpT[half]], [Bh])

    def proj_F(self, w_dram_cols, ncols, hT, Bh, col0, ntok, evac, wblk=512, psb=(0, 1, 2, 3), wbufs=None):
        p, ar = self.p, self.ar
        if wbufs is None:
            wb = [ar.alloc([KC, wblk], BF16) for _ in range(2)]
            Bw = p.bufs(2, "w")
        else:
            wb, Bw = wbufs
        nblk = (ncols + wblk - 1) // wblk
        k = 0
        for bi in range(nblk):
            b = bi % 2
            nc_ = min(wblk, ncols - bi * wblk)
            p.dma("pool", wb[b][:, :, :nc_], w_dram_cols(bi * wblk, nc_).rearrange("(c p) n -> p c n", p=128),
                  writes=[Bw[b]])
            for c in range(nc_ // 128):
                for t0 in range(0, ntok, 512):
                    nt = min(512, ntok - t0)
                    pi = psb[k % len(psb)]
                    k += 1

                    def mm(e, b=b, c=c, t0=t0, nt=nt, pi=pi):
                        r = None
                        for kc in range(KC):
                            r = e.matmul(self.ps[pi][:, :nt], lhsT=wb[b][:, kc, c * 128:(c + 1) * 128],
                                         rhs=hT[:, kc, col0 + t0:col0 + t0 + nt], start=(kc == 0), stop=(kc == KC - 1))
                        return r
                    p.op("pe", mm, reads=[Bw[b], Bh], writes=[self.Bps[pi]])
                    evac(bi * (wblk // 128) + c, t0, nt, self.ps[pi][:, :nt], self.Bps[pi])

    def qknorm(self, ps_ap, Bp, n, gvec, Bgv, out, Bout, scr):
        p = self.p
        sq, Bsq, rr, Brr, ssb = scr
        p.op("act", lambda e: e.activation(out=sq[:, :n], in_=ps_ap, func=AF.Square), reads=[Bp], writes=[Bsq])
        p.op("pe", lambda e: e.matmul(self.ps[ssb][:, :n], lhsT=self.ones, rhs=sq[:, :n], start=True, stop=True),
             reads=[Bsq, self.Bc], writes=[self.Bps[ssb]])
        p.op("act", lambda e: e.activation(out=rr[:, :n], in_=self.ps[ssb][:, :n], func=AF.Ln, bias=self.epsc, scale=1.0 / 128),
             reads=[self.Bps[ssb], self.Bc], writes=[Brr])
        p.op("act", lambda e: e.activation(out=rr[:, :n], in_=rr[:, :n], func=AF.Exp, scale=-0.5),
             reads=[Brr], writes=[Brr])
        p.op("dve", lambda e: e.scalar_tensor_tensor(out=out, in0=ps_ap, scalar=gvec, in1=rr[:, :n],
                                                     op0=ALU.mult, op1=ALU.mult),
             reads=[Bp, Brr, Bgv], writes=[Bout])

    def qk_scratch(self, ssb):
        ar, p = self.ar, self.p
        return (ar.alloc([512], BF16), p.buf(), ar.alloc([512], F32), p.buf(), ssb)

    def load_gvec(self, g_dram_row, scale):
        p = self.p
        gv = self.car.alloc([1], F32)
        B = p.buf()
        p.dma("sp", gv, g_dram_row.rearrange("(p o) -> p o", o=1), writes=[B])
        if scale != 1.0:
            p.op("dve", lambda e: e.tensor_scalar(out=gv, in0=gv, scalar1=scale, scalar2=None, op0=ALU.mult),
                 reads=[B], writes=[B])
        return gv, B


def phase_fin(k, src_rows, g, wcols, cs, z, own_off, do_norm, hT, Bh):
    p, ar = k.p, k.ar
    if do_norm:
        hT = ar.alloc([KC, T], BF16)
        Bh = p.buf("hT")
        k.norm_tiles(src_rows, g, hT, [(i, i * 128) for i in range(16)], Bh)
        own_off = 0
    finT = ar.alloc([4, T], BF16)
    Bf = p.buf("finT")
    csb = ar.alloc([256], BF16)
    Bcs = p.buf()
    p.dma("pool", csb, cs, writes=[Bcs])

    def ev(ci, t0, nt, ps_ap, Bp):
        k.copy(k.ev_eng(), finT[:, ci, t0:t0 + nt], ps_ap, [Bp], [Bf])
    k.proj_F(wcols, 512, hT, Bh, own_off, T, ev)
    zs = [ar.alloc([4, 256], BF16) for _ in range(2)]
    Bz = p.bufs(2, "zs")
    outs = []
    for i in range(16):
        b = i % 2
        pz = [k.ps[4 + 2 * b][:], k.ps[5 + 2 * b][:]]
        Bpz = [k.Bps[4 + 2 * b], k.Bps[5 + 2 * b]]
        for hh in range(2):
            def mm(e, i=i, hh=hh, pz=pz):
                r = None
                for gg in range(2):
                    g_ = hh * 2 + gg
                    r = e.matmul(pz[hh][:, gg * 256:(gg + 1) * 256], lhsT=finT[:, g_, i * 128:(i + 1) * 128], rhs=csb,
                                 start=True, stop=True)
                return r
            p.op("pe", mm, reads=[Bf, Bcs], writes=[Bpz[hh]])
            k.copy("act" if hh else "dve", zs[b][:, hh * 2:(hh + 1) * 2, :],
                   pz[hh].rearrange("p (a b) -> p a b", b=256), [Bpz[hh]], [Bz[b]])
        outs.append(p.dma("sp", z[:, i * 128:(i + 1) * 128, :].rearrange("g t c -> t g c"), zs[b], reads=[Bz[b]]))
    return outs


def emit_layer(k, nc, layer, x, zall, W, out_dma):
    moe = (layer % 2 == 1)
    p, ar = k.p, k.ar
    mem = W["mem"]; ln_mix_g = W["ln_mix_g"]; w_in = W["w_in"]; na_q_g = W["na_q_g"]; na_k_g = W["na_k_g"]
    ttab = W["ttab"]; rowmask = W["rowmask"]; mem_ln_g = W["mem_ln_g"]; w_mem_kv = W["w_mem_kv"]
    mem_q_g = W["mem_q_g"]; mem_k_g = W["mem_k_g"]; w_f_out = W["w_f_out"]; w_na_out = W["w_na_out"]
    w_mem_out = W["w_mem_out"]; w_o = W["w_o"]; ln_ffn_g = W["ln_ffn_g"]; w128 = W["w128"]; mtab = W["mtab"]
    wg_d = W["wg"]; wu_d = W["wu"]; wd_d = W["wd"]
    NE = (_DBG_NE or 8) if moe else 1
    if moe:
        wr_d = W["wr"]
    ft_d = W["ft_d"]; na_d = W["na_d"]; mo_d = W["mo_d"]; gates_d = W["gates_d"]; xmid_d = W["xmid_d"]
    SC = 1.0 / math.sqrt(128.0)

    k.phase()
    hT = ar.alloc([KC, TE], BF16)
    Bh = p.buf("hT")
    mark_h = ar.off
    k.norm_tiles(lambda i: x[i * 128:(i + 1) * 128, :], ln_mix_g, hT, [(i, i * 128) for i in range(20)], Bh)

    k.phase()
    ar.off = mark_h
    gs = [ar.alloc([512], F32) for _ in range(4)]
    Bgs = p.bufs(4, "gs")
    cnt = [0]

    def ev_g(ci, t0, nt, ps_ap, Bp):
        b = cnt[0] % 4
        cnt[0] += 1
        p.op("act", lambda e: e.activation(out=gs[b][:, :nt], in_=ps_ap, func=AF.Sigmoid), reads=[Bp], writes=[Bgs[b]])
        p.dma("sp", gates_d[ci, :, t0:t0 + nt], gs[b][:, :nt], reads=[Bgs[b]])
    k.proj_F(lambda c0, n: w_in[:, 4096 + c0:4096 + c0 + n], 6144, hT, Bh, 256, T, ev_g)

    k.phase()
    ar.off = mark_h
    mhT = ar.alloc([KC, 256], BF16)
    Bmh = p.buf("mhT")
    mark_m = ar.off
    k.norm_tiles(lambda i: mem[i * 128:(i + 1) * 128, :], mem_ln_g, mhT, [(0, 0), (1, 128)], Bmh)
    k.phase()
    ar.off = mark_m
    gq_m, Bgqm = k.load_gvec(mem_q_g, SC)
    gk_m, Bgkm = k.load_gvec(mem_k_g, 1.0)
    kTm = ar.alloc([4, 256], BF16)
    BkTm = p.buf("kTm")
    vm = ar.alloc([2, 512], BF16)
    Bvm = p.buf("vm")
    scr = k.qk_scratch(4)

    def ev_km(ci, t0, nt, ps_ap, Bp):
        k.qknorm(ps_ap, Bp, nt, gk_m, Bgkm, kTm[:, ci, t0:t0 + nt], BkTm, scr)
    k.proj_F(lambda c0, n: w_mem_kv[:, c0:c0 + n], 512, mhT, Bmh, 0, 256, ev_km)
    wv = ar.alloc([KC, 512], BF16)
    Bwv = p.buf()
    p.dma("pool", wv, w_mem_kv[:, 512:1024].rearrange("(c p) n -> p c n", p=128), writes=[Bwv])
    for mt_ in range(2):
        def mmv(e, mt_=mt_):
            r = None
            for kc in range(KC):
                r = e.matmul(k.ps[mt_][:], lhsT=mhT[:, kc, mt_ * 128:(mt_ + 1) * 128], rhs=wv[:, kc, :],
                             start=(kc == 0), stop=(kc == KC - 1))
            return r
        p.op("pe", mmv, reads=[Bmh, Bwv], writes=[k.Bps[mt_]])
        k.copy(k.ev_eng(), vm[:, mt_, :], k.ps[mt_][:], [k.Bps[mt_]], [Bvm])
    qmT = ar.alloc([T], BF16)
    BqmT = p.buf("qmT")
    pT_ = [ar.alloc([2, 512], BF16) for _ in range(2)]
    BpT_ = p.bufs(2, "pTm")
    rs = ar.alloc([512], F32)
    Brs = p.buf()
    mo_s = ar.alloc([T], BF16)
    Bmo = p.buf("mo_s")
    wqb = ([ar.alloc([KC, 128], BF16) for _ in range(2)], p.bufs(2, "wq"))
    for h in range(4):
        def ev_qm(ci, t0, nt, ps_ap, Bp):
            k.qknorm(ps_ap, Bp, nt, gq_m, Bgqm, qmT[:, t0:t0 + nt], BqmT, scr)
        k.proj_F(lambda c0, n, h=h: w_in[:, 3584 + h * 128 + c0:3584 + h * 128 + c0 + n], 128, hT, Bh, 256, T,
                 ev_qm, wblk=128, psb=(0, 1), wbufs=wqb)
        for tt in range(4):
            b = tt % 2
            for m2 in range(2):
                pi = 2 + m2
                p.op("pe", lambda e, m2=m2, tt=tt, h=h, pi=pi: e.matmul(
                    k.ps[pi][:], lhsT=kTm[:, h, m2 * 128:(m2 + 1) * 128], rhs=qmT[:, tt * 512:(tt + 1) * 512],
                    start=True, stop=True), reads=[BkTm, BqmT], writes=[k.Bps[pi]])
                p.op("act", lambda e, m2=m2, b=b, pi=pi: e.activation(out=pT_[b][:, m2, :], in_=k.ps[pi][:], func=AF.Exp),
                     reads=[k.Bps[pi]], writes=[BpT_[b]])

            def mo_mm(e, b=b, h=h):
                r = None
                for m2 in range(2):
                    r = e.matmul(k.ps[5][:], lhsT=vm[:, m2, h * 128:(h + 1) * 128], rhs=pT_[b][:, m2, :],
                                 start=(m2 == 0), stop=(m2 == 1))
                return r
            p.op("pe", mo_mm, reads=[Bvm, BpT_[b]], writes=[k.Bps[5]])

            def sm_mm(e, b=b):
                r = None
                for m2 in range(2):
                    r = e.matmul(k.ps[6][:], lhsT=k.ones, rhs=pT_[b][:, m2, :], start=(m2 == 0), stop=(m2 == 1))
                return r
            p.op("pe", sm_mm, reads=[k.Bc, BpT_[b]], writes=[k.Bps[6]])
            p.op("dve", lambda e: e.reciprocal(out=rs, in_=k.ps[6][:]), reads=[k.Bps[6]], writes=[Brs])
            p.op("dve", lambda e, tt=tt: e.tensor_tensor(out=mo_s[:, tt * 512:(tt + 1) * 512], in0=k.ps[5][:], in1=rs,
                                                         op=ALU.mult), reads=[k.Bps[5], Brs], writes=[Bmo])
        p.dma("sp", mo_d[h], mo_s, reads=[Bmo])

    k.phase()
    ar.off = mark_h
    gq_n, Bgqn = k.load_gvec(na_q_g, SC)
    gk_n, Bgkn = k.load_gvec(na_k_g, 1.0)
    rm = ar.alloc([5, 768], F32)
    Brm = p.buf("rowmask")
    p.dma("sp", rm, rowmask.rearrange("v k j q -> k v (j q)"), writes=[Brm])
    bias = ar.alloc([5, 768], F32)
    Bbias = p.buf("bias")
    p.op("pool", lambda e: e.memset(bias, 0.0), writes=[Bbias])
    scr = k.qk_scratch(4)
    qT = ar.alloc([T], BF16)
    BqT = p.buf("qT")
    kT = ar.alloc([TE], BF16)
    BkT = p.buf("kT")
    vh = ar.alloc([20, 128], BF16)
    Bvh = p.buf("vh")
    wvh = ar.alloc([KC, 128], BF16)
    Bwvh = p.buf("wvh")
    e1 = [ar.alloc([6, 128], F32) for _ in range(2)]
    Be1 = p.bufs(2, "e1")
    pt = [ar.alloc([6, 128], BF16) for _ in range(2)]
    Bpt = p.bufs(2, "pt")
    rsn = ar.alloc([128], F32)
    Brsn = p.buf()
    na_s = ar.alloc([T], BF16)
    Bna = p.buf("na_s")
    wqb = ([ar.alloc([KC, 128], BF16) for _ in range(2)], p.bufs(2, "wqn"))

    def variant(m):
        return {0: 0, 1: 1, 14: 3, 15: 4}.get(m, 2)

    def tiles_of(m):
        if m == 0:
            return list(range(0, 6))
        if m == 15:
            return list(range(14, 20))
        return list(range(m, m + 5))
    rep_m = {0: 0, 1: 1, 2: 2, 3: 14, 4: 15}
    for h in range(8):
        fresh = []
        p.op("pool", lambda e: e.nop(), writes=[Bbias])
        for v in range(5):
            m = rep_m[v]
            for jj, j in enumerate(tiles_of(m)):
                for kr in range(2):
                    dr0 = (2 * j - 4 + kr) - (2 * m)
                    lo, hi = 7 - dr0, 7 - dr0 + 1
                    for qr, ri in ((0, lo), (1, hi)):
                        if 0 <= ri <= 14:
                            fb_ = p.buf()
                            fresh.append(fb_)
                            p.dma("sp", bias[kr * 64:(kr + 1) * 64, v, jj * 128 + qr * 64: jj * 128 + qr * 64 + 64],
                                  ttab[h, ri], reads=[Bbias], writes=[fb_])
        p.op("pool", lambda e: e.tensor_tensor(out=bias, in0=bias, in1=rm, op=ALU.add), reads=[Brm] + fresh, writes=[Bbias])

        def ev_q(ci, t0, nt, ps_ap, Bp):
            k.qknorm(ps_ap, Bp, nt, gq_n, Bgqn, qT[:, t0:t0 + nt], BqT, scr)
        k.proj_F(lambda c0, n, h=h: w_in[:, 512 + h * 128 + c0:512 + h * 128 + c0 + n], 128, hT, Bh, 256, T, ev_q,
                 wblk=128, psb=(0, 1), wbufs=wqb)

        def ev_k(ci, t0, nt, ps_ap, Bp):
            k.qknorm(ps_ap, Bp, nt, gk_n, Bgkn, kT[:, t0:t0 + nt], BkT, scr)
        k.proj_F(lambda c0, n, h=h: w_in[:, 1536 + h * 128 + c0:1536 + h * 128 + c0 + n], 128, hT, Bh, 0, TE, ev_k,
                 wblk=128, psb=(0, 1), wbufs=wqb)
        p.dma("pool", wvh, w_in[:, 2560 + h * 128:2560 + (h + 1) * 128].rearrange("(c p) n -> p c n", p=128),
              writes=[Bwvh])
        for i4 in range(5):
            pi = 2 + (i4 % 2)

            def mmv(e, i4=i4, pi=pi):
                r = None
                for j in range(4):
                    i = i4 * 4 + j
                    for kc in range(KC):
                        r = e.matmul(k.ps[pi][:, j * 128:(j + 1) * 128], lhsT=hT[:, kc, i * 128:(i + 1) * 128],
                                     rhs=wvh[:, kc, :], start=(kc == 0), stop=(kc == KC - 1))
                return r
            p.op("pe", mmv, reads=[Bh, Bwvh], writes=[k.Bps[pi]])
            k.copy(k.ev_eng(), vh[:, i4 * 4:(i4 + 1) * 4, :], k.ps[pi][:].rearrange("p (a b) -> p a b", b=128),
                   [k.Bps[pi]], [Bvh])
        for m in range(16):
            b = m % 2
            v = variant(m)
            tl = tiles_of(m)
            NT = len(tl)
            pa, pb_ = (4, 5) if b == 0 else (6, 7)

            def sc(e, m=m, tl=tl, pa=pa, pb_=pb_):
                r = None
                for jj, j in enumerate(tl):
                    dst = k.ps[pa][:, jj * 128:(jj + 1) * 128] if jj < 4 else k.ps[pb_][:, (jj - 4) * 128:(jj - 3) * 128]
                    r = e.matmul(dst, lhsT=kT[:, j * 128:(j + 1) * 128], rhs=qT[:, m * 128:(m + 1) * 128],
                                 start=True, stop=True)
                return r
            p.op("pe", sc, reads=[BkT, BqT], writes=[k.Bps[pa], k.Bps[pb_]])
            p.op("dve", lambda e, b=b, v=v, pa=pa: e.tensor_tensor(
                out=e1[b][:, 0:4, :], in0=k.ps[pa][:].rearrange("p (a b) -> p a b", b=128),
                in1=bias[:, v, 0:512].rearrange("p (a b) -> p a b", b=128), op=ALU.add),
                reads=[k.Bps[pa], Bbias], writes=[Be1[b]])
            p.op("dve", lambda e, b=b, v=v, pb_=pb_, NT=NT: e.tensor_tensor(
                out=e1[b][:, 4:NT, :], in0=k.ps[pb_][:, 0:(NT - 4) * 128].rearrange("p (a b) -> p a b", b=128),
                in1=bias[:, v, 512:NT * 128].rearrange("p (a b) -> p a b", b=128), op=ALU.add),
                reads=[k.Bps[pb_], Bbias], writes=[Be1[b]])
            p.op("act", lambda e, b=b, NT=NT: e.activation(out=pt[b][:, 0:NT, :], in_=e1[b][:, 0:NT, :], func=AF.Exp),
                 reads=[Be1[b]], writes=[Bpt[b]])
            po = 2 + b

            def ov(e, b=b, tl=tl, po=po):
                r = None
                for jj, j in enumerate(tl):
                    r = e.matmul(k.ps[po][:, 0:128], lhsT=vh[:, j, :], rhs=pt[b][:, jj, :], start=(jj == 0),
                                 stop=(jj == len(tl) - 1))
                for jj, j in enumerate(tl):
                    r = e.matmul(k.ps[po][:, 128:256], lhsT=k.ones, rhs=pt[b][:, jj, :], start=(jj == 0),
                                 stop=(jj == len(tl) - 1))
                return r
            p.op("pe", ov, reads=[Bvh, Bpt[b], k.Bc], writes=[k.Bps[po]])
            p.op("dve", lambda e, po=po: e.reciprocal(out=rsn, in_=k.ps[po][:, 128:256]), reads=[k.Bps[po]], writes=[Brsn])
            p.op("dve", lambda e, po=po, m=m: e.tensor_tensor(out=na_s[:, m * 128:(m + 1) * 128], in0=k.ps[po][:, 0:128],
                                                              in1=rsn, op=ALU.mult),
                 reads=[k.Bps[po], Brsn], writes=[Bna])
        p.dma("sp", na_d[h], na_s, reads=[Bna])

    k.phase()
    w128s = ar.alloc([512], BF16)
    Bw128 = p.buf()
    p.dma("pool", w128s, w128, writes=[Bw128])
    mt = ar.alloc([128, 2, 16], BF16)
    Bmt = p.buf()
    p.dma("pool", mt, mtab, writes=[Bmt])
    zg = ar.alloc([128, 256], BF16)
    Bzg = p.buf("zg")
    ag = ar.alloc([128, 256], BF16)
    Bag = p.buf("ag")
    ftg = [ar.alloc([T], BF16) for _ in range(2)]
    Bftg = p.bufs(2, "ftg")
    fsc = 1.0 / math.sqrt(16384.0 * 128.0)
    for g_ in range(4):
        for r in range(NCORES):
            p.dma("sp", zg[16 * r:16 * (r + 1)], zall[r, g_].rearrange("(t n) c -> t n c", n=128), writes=[Bzg])
        for c2 in range(64):
            pi = c2 % 4

            def s1(e, c2=c2, pi=pi):
                r = None
                for j in range(2):
                    ch = c2 * 2 + j
                    e.matmul(k.ps[pi][:, j * 256:(j + 1) * 256], lhsT=zg[:, :, ch], rhs=w128s[:, 0:256],
                             start=True, stop=False)
                    r = e.matmul(k.ps[pi][:, j * 256:(j + 1) * 256], lhsT=zg[:, :, 128 + ch], rhs=w128s[:, 256:512],
                                 start=False, stop=True)
                return r
            p.op("pe", s1, reads=[Bzg, Bw128], writes=[k.Bps[pi]])
            k.copy(k.ev_eng(), ag[:, c2 * 2:c2 * 2 + 2, :], k.ps[pi][:].rearrange("p (a b) -> p a b", b=256),
                   [k.Bps[pi]], [Bag])
        for q4 in range(4):
            pi = 4 + q4

            def s3(e, q4=q4, pi=pi):
                r = None
                for kk in range(32):
                    k1 = q4 * 32 + kk
                    e.matmul(k.ps[pi][:, kk * 16:(kk + 1) * 16], lhsT=ag[:, :, k1], rhs=mt[:, k1, 0, :],
                             start=True, stop=False)
                    r = e.matmul(k.ps[pi][:, kk * 16:(kk + 1) * 16], lhsT=ag[:, :, 128 + k1], rhs=mt[:, k1, 1, :],
                                 start=False, stop=True)
                return r
            p.op("pe", s3, reads=[Bag, Bmt], writes=[k.Bps[pi]])
            fb = g_ % 2
            outv = ftg[fb].rearrange("p (k2 k1) -> p k2 k1", k1=128)[:, :, q4 * 32:(q4 + 1) * 32]
            inv = k.ps[pi][:].rearrange("p (kk k2) -> p k2 kk", k2=16)
            p.op("act" if q4 % 2 else "dve",
                 (lambda e, outv=outv, inv=inv: e.activation(out=outv, in_=inv, func=AF.Copy, scale=fsc)) if q4 % 2 else
                 (lambda e, outv=outv, inv=inv: e.tensor_scalar(out=outv, in0=inv, scalar1=fsc, scalar2=None, op0=ALU.mult)),
                 reads=[k.Bps[pi]], writes=[Bftg[fb]])
        p.dma("sp", ft_d[g_], ftg[g_ % 2], reads=[Bftg[g_ % 2]])

    k.phase()
    srcT = ar.alloc([KC, 512], BF16)
    Bsrc = p.buf("srcT")
    wc = [ar.alloc([KC, 512], BF16) for _ in range(2)]
    Bwc = p.bufs(2, "wc")
    gt = [ar.alloc([3, 512], F32) for _ in range(2)]
    Bgt = p.bufs(2, "gt")
    t3 = [ar.alloc([512], F32) for _ in range(3)]
    Bt3 = p.bufs(3, "t3")
    mT = ar.alloc([KC, 512], BF16)
    BmT = p.buf("mT")
    wo = [ar.alloc([KC, 512], BF16) for _ in range(2)]
    Bwo = p.bufs(2, "wo")
    xin = [ar.alloc([512], F32) for _ in range(2)]
    Bxin = p.bufs(2, "xin")
    xo = [ar.alloc([512], F32) for _ in range(2)]
    Bxo = p.bufs(2, "xo")
    for tt in range(4):
        tsl = slice(tt * 512, (tt + 1) * 512)
        p.dma("sp", srcT[:, 0:4, :], ft_d[:, :, tsl].rearrange("g p t -> p g t"), writes=[Bsrc])
        p.dma("sp", srcT[:, 4:12, :], na_d[:, :, tsl].rearrange("g p t -> p g t"), writes=[Bsrc])
        p.dma("sp", srcT[:, 12:16, :], mo_d[:, :, tsl].rearrange("g p t -> p g t"), writes=[Bsrc])
        for nb in range(4):
            b = nb % 2
            csl = slice(nb * 512, (nb + 1) * 512)
            p.dma("pool", wc[b][:, 0:4, :], w_f_out[:, csl].rearrange("(c p) n -> p c n", p=128), writes=[Bwc[b]])
            p.dma("pool", wc[b][:, 4:12, :], w_na_out[:, csl].rearrange("(c p) n -> p c n", p=128), writes=[Bwc[b]])
            p.dma("pool", wc[b][:, 12:16, :], w_mem_out[:, csl].rearrange("(c p) n -> p c n", p=128), writes=[Bwc[b]])
            for c in range(4):
                n = nb * 4 + c
                gb = n % 2
                p.dma("sp", gt[gb], gates_d[:, :, tsl].rearrange("(b n) p t -> n p b t", b=3)[n], writes=[Bgt[gb]])
                rng = ((0, 4), (4, 12), (12, 16))
                for br in range(3):
                    def mm(e, br=br, b=b, c=c):
                        r = None
                        lo, hi = rng[br]
                        for kc in range(lo, hi):
                            r = e.matmul(k.ps[br][:], lhsT=wc[b][:, kc, c * 128:(c + 1) * 128], rhs=srcT[:, kc, :],
                                         start=(kc == lo), stop=(kc == hi - 1))
                        return r
                    p.op("pe", mm, reads=[Bwc[b], Bsrc], writes=[k.Bps[br]])
                    p.op("dve", lambda e, br=br, gb=gb: e.tensor_tensor(out=t3[br], in0=k.ps[br][:], in1=gt[gb][:, br, :],
                                                                        op=ALU.mult),
                         reads=[k.Bps[br], Bgt[gb]], writes=[Bt3[br]])
                p.op("pool", lambda e: e.tensor_tensor(out=t3[0], in0=t3[0], in1=t3[1], op=ALU.add),
                     reads=[Bt3[0], Bt3[1]], writes=[Bt3[0]])
                p.op("pool", lambda e, n=n: e.tensor_tensor(out=mT[:, n, :], in0=t3[0], in1=t3[2], op=ALU.add),
                     reads=[Bt3[0], Bt3[2]], writes=[BmT])
        for nb in range(4):
            b = nb % 2
            csl = slice(nb * 512, (nb + 1) * 512)
            p.dma("pool", wo[b], w_o[:, csl].rearrange("(c p) n -> p c n", p=128), writes=[Bwo[b]])
            for s in range(4):
                xb = (nb * 4 + s) % 2
                pi = 4 + xb
                tok0 = tt * 512 + s * 128
                p.dma("sp", xin[xb], x[256 + tok0:256 + tok0 + 128, csl], writes=[Bxin[xb]])

                def mm(e, b=b, s=s, pi=pi):
                    r = None
                    for kc in range(KC):
                        r = e.matmul(k.ps[pi][:], lhsT=mT[:, kc, s * 128:(s + 1) * 128], rhs=wo[b][:, kc, :],
                                     start=(kc == 0), stop=(kc == KC - 1))
                    return r
                p.op("pe", mm, reads=[BmT, Bwo[b]], writes=[k.Bps[pi]])
                p.op("dve", lambda e, xb=xb, pi=pi: e.tensor_tensor(out=xo[xb], in0=k.ps[pi][:], in1=xin[xb], op=ALU.add),
                     reads=[k.Bps[pi], Bxin[xb]], writes=[Bxo[xb]])
                p.dma("sp", xmid_d[tok0:tok0 + 128, csl], xo[xb], reads=[Bxo[xb]])

    k.phase()
    FH = (16, 16, 12)
    h2T = ar.alloc([KC, 512], BF16)
    Bh2 = p.buf("h2T")
    yacc = ar.alloc([4, D], F32)
    By = p.buf("yacc")
    actT = ar.alloc([16, 512], BF16)
    Bact = p.buf("actT")
    wgu = [[ar.alloc([KC, 512], BF16) for _ in range(2)] for _ in range(2)]
    Bwgu = [p.bufs(2, "wg"), p.bufs(2, "wu")]
    wdn = [ar.alloc([16, 512], BF16) for _ in range(2)]
    Bwdn = p.bufs(2, "wdn")
    sl = [ar.alloc([512], F32) for _ in range(2)]
    Bsl = p.bufs(2, "sl")
    if moe:
        wr = ar.alloc([KC, 8], F32)
        Bwr = p.buf("wr")
        p.dma("sp", wr, wr_d.rearrange("(c p) n -> p c n", p=128), writes=[Bwr])
        lg = ar.alloc([8], F32)
        Blg_s = p.buf()
        m8 = ar.alloc([8], F32)
        Bm8 = p.buf()
        sc4 = ar.alloc([4], F32)
        Bsc4 = p.buf()
        msk = [ar.alloc([8], F32) for _ in range(2)]
        Bmsk = p.bufs(2, "msk")
        wts = ar.alloc([4, 8], F32)
        Bwts = p.buf("wts")
    mark_f = ar.off
    outs = []
    for qt in range(4):
        if qt:
            p.barrier()
        ar.off = mark_f

        def keep(n, xt_ap, Bxt):
            p.op("pool", lambda e: e.tensor_copy(out=yacc[:, n, :], in_=xt_ap), reads=[Bxt], writes=[By])
        router = None
        if moe:
            def done(n):
                p.op("dve", lambda e: e.tensor_copy(out=lg, in_=k.ps[4][:, 0:8]), reads=[k.Bps[4]], writes=[Blg_s])
                p.op("dve", lambda e: e.max(out=m8, in_=lg), reads=[Blg_s], writes=[Bm8])
                p.op("dve", lambda e: e.tensor_scalar(out=sc4[:, 0:1], in0=m8[:, 0:1], scalar1=-1.0, scalar2=None, op0=ALU.mult),
                     reads=[Bm8], writes=[Bsc4])
                p.op("act", lambda e: e.activation(out=sc4[:, 1:2], in_=m8[:, 1:2], func=AF.Exp, bias=sc4[:, 0:1], scale=1.0),
                     reads=[Bm8, Bsc4], writes=[Bsc4])
                p.op("dve", lambda e: e.tensor_scalar(out=sc4[:, 2:3], in0=sc4[:, 1:2], scalar1=1.0, scalar2=None, op0=ALU.add),
                     reads=[Bsc4], writes=[Bsc4])
                p.op("dve", lambda e: e.reciprocal(out=sc4[:, 2:3], in_=sc4[:, 2:3]), reads=[Bsc4], writes=[Bsc4])
                p.op("dve", lambda e: e.tensor_tensor(out=sc4[:, 3:4], in0=sc4[:, 1:2], in1=sc4[:, 2:3], op=ALU.mult),
                     reads=[Bsc4], writes=[Bsc4])
                p.op("dve", lambda e: e.tensor_scalar(out=msk[0], in0=lg, scalar1=m8[:, 0:1], scalar2=None, op0=ALU.is_equal),
                     reads=[Blg_s, Bm8], writes=[Bmsk[0]])
                p.op("dve", lambda e: e.tensor_scalar(out=msk[1], in0=lg, scalar1=m8[:, 1:2], scalar2=None, op0=ALU.is_equal),
                     reads=[Blg_s, Bm8], writes=[Bmsk[1]])
                p.op("dve", lambda e: e.tensor_scalar(out=wts[:, n, :], in0=msk[0], scalar1=sc4[:, 2:3], scalar2=None, op0=ALU.mult),
                     reads=[Bmsk[0], Bsc4], writes=[Bwts])
                p.op("dve", lambda e: e.scalar_tensor_tensor(out=wts[:, n, :], in0=msk[1], scalar=sc4[:, 3:4], in1=wts[:, n, :],
                                                             op0=ALU.mult, op1=ALU.add),
                     reads=[Bmsk[1], Bsc4, Bwts], writes=[Bwts])
            router = (wr, Bwr, k.ps[4][:, 0:8], k.Bps[4], done)
        k.norm_tiles(lambda i: xmid_d[i * 128:(i + 1) * 128, :], ln_ffn_g, h2T,
                     [(qt * 4 + n, n * 128) for n in range(4)], Bh2, keep=keep, router=router, lean=True)
        for ex in range(NE):
            ff0 = 0
            for half in range(len(FH)):
                nch = FH[half]
                for blk in range(nch // 4):
                    b = blk % 2
                    c0 = (ff0 + blk * 4) * 128
                    p.dma("pool", wgu[0][b], wg_d[ex][:, c0:c0 + 512].rearrange("(c p) n -> p c n", p=128), writes=[Bwgu[0][b]])
                    p.dma("pool", wgu[1][b], wu_d[ex][:, c0:c0 + 512].rearrange("(c p) n -> p c n", p=128), writes=[Bwgu[1][b]])
                    for c in range(4):
                        sb_ = (blk * 4 + c) % 2
                        pg, pu = (0, 1) if sb_ == 0 else (2, 3)
                        for gu, pi in ((0, pg), (1, pu)):
                            def mm(e, gu=gu, pi=pi, b=b, c=c):
                                r = None
                                for kc in range(KC):
                                    r = e.matmul(k.ps[pi][:], lhsT=wgu[gu][b][:, kc, c * 128:(c + 1) * 128], rhs=h2T[:, kc, :],
                                                 start=(kc == 0), stop=(kc == KC - 1))
                                return r
                            p.op("pe", mm, reads=[Bwgu[gu][b], Bh2], writes=[k.Bps[pi]])
                        p.op("act", lambda e, sb_=sb_, pg=pg: e.activation(out=sl[sb_], in_=k.ps[pg][:], func=AF.Silu),
                             reads=[k.Bps[pg]], writes=[Bsl[sb_]])
                        p.op("dve", lambda e, sb_=sb_, pu=pu, ci=blk * 4 + c: e.tensor_tensor(
                            out=actT[:, ci, :], in0=k.ps[pu][:], in1=sl[sb_], op=ALU.mult),
                            reads=[k.Bps[pu], Bsl[sb_]], writes=[Bact])
                for nb in range(4):
                    b = nb % 2
                    p.dma("pool", wdn[b][:, 0:nch, :],
                          wd_d[ex][ff0 * 128:(ff0 + nch) * 128, nb * 512:(nb + 1) * 512].rearrange("(c p) n -> p c n", p=128),
                          writes=[Bwdn[b]])
                    for s in range(4):
                        pi = 4 + (nb * 4 + s) % 2 if not moe else 5 + (nb * 4 + s) % 2
                        pi = 5 + (nb * 4 + s) % 2

                        def mm(e, b=b, s=s, pi=pi, nch=nch):
                            r = None
                            for ci in range(nch):
                                r = e.matmul(k.ps[pi][:, 0:512], lhsT=actT[:, ci, s * 128:(s + 1) * 128], rhs=wdn[b][:, ci, :],
                                             start=(ci == 0), stop=(ci == nch - 1))
                            return r
                        p.op("pe", mm, reads=[Bact, Bwdn[b]], writes=[k.Bps[pi]])
                        ysl = yacc[:, s, nb * 512:(nb + 1) * 512]
                        if moe:
                            p.op("dve", lambda e, pi=pi, ysl=ysl, s=s, ex=ex: e.scalar_tensor_tensor(
                                out=ysl, in0=k.ps[pi][:, 0:512], scalar=wts[:, s, ex:ex + 1], in1=ysl, op0=ALU.mult, op1=ALU.add),
                                reads=[k.Bps[pi], Bwts, By], writes=[By])
                        else:
                            p.op("dve", lambda e, pi=pi, ysl=ysl: e.tensor_tensor(out=ysl, in0=k.ps[pi][:, 0:512], in1=ysl, op=ALU.add),
                                 reads=[k.Bps[pi], By], writes=[By])
                ff0 += nch
        for s in range(4):
            tok0 = qt * 512 + s * 128
            outs.extend(out_dma(tok0, yacc[:, s, :], By))
    return outs


def _dft_consts():
    c = np.arange(128)
    ang = 2.0 * np.pi * np.outer(c, c) / 128.0
    C = np.cos(ang)
    S = np.sin(ang)
    cs = np.concatenate([C, -S], axis=1).astype(np.float32)
    w128 = np.concatenate([C, S, S, -C], axis=1).astype(np.float32)
    mt = []
    n2 = np.arange(128)[:, None, None]
    k1 = np.arange(128)[None, :, None]
    for core in range(NCORES):
        k2 = (np.arange(16) + 16 * core)[None, None, :]
        kk = (k1 + 128 * k2)
        a = 2.0 * np.pi * ((n2 * kk) % 16384) / 16384.0
        mt.append(np.stack([np.cos(a), -np.sin(a)], axis=2).astype(np.float32))
    return cs, w128, mt


def _na_tables(rpb):
    cols = np.arange(64)
    cstart = np.clip(cols - 8, 0, 48)
    kc = np.arange(64)[:, None]
    qc = np.arange(64)[None, :]
    valid = (kc >= cstart[None, :]) & (kc < cstart[None, :] + 16)
    off = np.clip(kc - qc + 15, 0, 30)
    tt = np.full((8, 16, 64, 64), NEG, np.float32)
    for ri in range(15):
        dr = 7 - ri
        g = rpb[:, dr + 7][:, off]
        tt[:, ri] = np.where(valid[None], g, np.float32(NEG))
    return tt


def _rowmask(core):
    rm = np.full((5, 128, 6, 128), NEG, np.float32)
    rep_m = {0: 0, 1: 1, 2: 2, 3: 14, 4: 15}
    for v in range(5):
        m = rep_m[v]
        tl = list(range(0, 6)) if m == 0 else (list(range(14, 20)) if m == 15 else list(range(m, m + 5)))
        for jj, j in enumerate(tl):
            for kr in range(2):
                krow = 32 * core + (2 * j - 4 + kr)
                for qr in range(2):
                    qrow = 32 * core + 2 * m + qr
                    rs = min(max(qrow - 4, 0), 256 - 8)
                    if rs <= krow < rs + 8:
                        rm[v, kr * 64:(kr + 1) * 64, jj, qr * 64:(qr + 1) * 64] = 0.0
    return rm


_CACHE = {}
_DBG_NE = 0
_DBG_L = 2
FUSED = False


def build_fused():
    nc = bass.Bass("TRN2", target_bir_lowering=False)
    I32 = mybir.dt.int32
    din = lambda name, shape, dt=F32: nc.dram_tensor(name, shape, dt, kind="ExternalInput").ap()
    x0 = din("x", [TE, D])
    mem = din("mem", [256, D])
    ln_mix_g = din("ln_mix_g", [2, D])
    w_in = din("w_in", [2, D, 10240])
    na_q_g = din("na_q_g", [2, 128])
    na_k_g = din("na_k_g", [2, 128])
    ttab = din("ttab", [2, 8, 16, 64, 64])
    rowmask = din("rowmask", [5, 128, 6, 128])
    mem_ln_g = din("mem_ln_g", [2, D])
    w_mem_kv = din("w_mem_kv", [2, D, 1024])
    mem_q_g = din("mem_q_g", [2, 128])
    mem_k_g = din("mem_k_g", [2, 128])
    w_f_out = din("w_fourier_out", [2, 512, D])
    w_na_out = din("w_na_out", [2, 1024, D])
    w_mem_out = din("w_mem_out", [2, 512, D])
    w_o = din("w_o", [2, D, D])
    ln_ffn_g = din("ln_ffn_g", [2, D])
    cs = din("cs", [128, 256])
    w128 = din("w128", [128, 512])
    mtab = din("mtab", [128, 128, 2, 16])
    hidx = din("hidx", [128, 4], I32)
    ffn_wg = din("ffn_w_gate", [1, D, DFF])
    ffn_wu = din("ffn_w_up", [1, D, DFF])
    ffn_wd = din("ffn_w_down", [1, DFF, D])
    wr_d = din("moe_router", [D, 8])
    moe_wg = din("moe_w_gate", [8, D, DFF])
    moe_wu = din("moe_w_up", [8, D, DFF])
    moe_wd = din("moe_w_down", [8, DFF, D])
    xout = nc.dram_tensor("xout", [T, D], F32, kind="ExternalOutput").ap()
    dsc = lambda name, shape, dt, **kw: nc.dram_tensor(name, shape, dt, **kw)
    scratch = {"ft_d": dsc("ft_d", [4, 128, T], BF16).ap(), "na_d": dsc("na_d", [8, 128, T], BF16).ap(),
               "mo_d": dsc("mo_d", [4, 128, T], BF16).ap(), "gates_d": dsc("gates_d", [48, 128, T], F32).ap(),
               "xmid_d": dsc("xmid_d", [T, D], F32).ap()}
    z_own = [dsc(f"z_own{l}", [4 * T, 256], BF16) for l in range(2)]
    z_all = [dsc(f"z_all{l}", [NCORES * 4 * T, 256], BF16, addr_space="Shared") for l in range(2)]
    x1 = dsc("x1_ext", [TE, D], F32).ap()
    edge_own = dsc("edge_own", [512, D], F32)
    edge_all = dsc("edge_all", [NCORES * 512, D], F32, addr_space="Shared")
    with ExitStack() as st:
        k = K(nc, st)
        p, ar = k.p, k.ar
        k.consts()
        final = []
        for l in range(_DBG_L):
            xl = x0 if l == 0 else x1
            if l == 1:
                k.phase()
                Bcc = p.buf("edge_all")
                p.cc("AllGather", [edge_own.ap().opt()], [edge_all.ap().opt()], writes=[Bcc])
                idx = ar.alloc([4], I32)
                Bidx = p.buf()
                p.dma("sp", idx, hidx, writes=[Bidx])
                ht = [ar.alloc([D], F32) for _ in range(2)]
                Bht = p.bufs(2, "ht")
                for j in range(4):
                    b = j % 2
                    def gat(e, j=j, b=b):
                        return e.indirect_dma_start(out=ht[b], out_offset=None, in_=edge_all.ap(),
                                                    in_offset=bass.IndirectOffsetOnAxis(ap=idx[:, j:j + 1], axis=0))
                    p.op("pool", gat, reads=[Bcc, Bidx], writes=[Bht[b]], is_dma=True)
                    row0 = j * 128 if j < 2 else 2304 + (j - 2) * 128
                    p.dma("sp", x1[row0:row0 + 128, :], ht[b], reads=[Bht[b]])
            k.phase()
            zo = z_own[l].ap().rearrange("(g t) c -> g t c", g=4)
            phase_fin(k, lambda i, xl=xl: xl[256 + i * 128:256 + (i + 1) * 128, :], ln_mix_g[l],
                      lambda c0, n, l=l: w_in[l][:, c0:c0 + n], cs, zo, own_off=0, do_norm=True, hT=None, Bh=None)
            k.phase()
            Bza = p.buf("zall")
            p.cc("AllGather", [z_own[l].ap().opt()], [z_all[l].ap().opt()], writes=[Bza])
            zall = z_all[l].ap().rearrange("(r g t) c -> r g t c", r=NCORES, g=4)
            W = {"mem": mem, "ln_mix_g": ln_mix_g[l], "w_in": w_in[l], "na_q_g": na_q_g[l], "na_k_g": na_k_g[l],
                 "ttab": ttab[l], "rowmask": rowmask, "mem_ln_g": mem_ln_g[l], "w_mem_kv": w_mem_kv[l],
                 "mem_q_g": mem_q_g[l], "mem_k_g": mem_k_g[l], "w_f_out": w_f_out[l], "w_na_out": w_na_out[l],
                 "w_mem_out": w_mem_out[l], "w_o": w_o[l], "ln_ffn_g": ln_ffn_g[l], "w128": w128, "mtab": mtab}
            W.update(scratch)
            if l == 0 and _DBG_L == 1:
                W.update({"wg": ffn_wg, "wu": ffn_wu, "wd": ffn_wd})

                def out_dma(tok0, src, Bsrc):
                    return [p.dma("sp", xout[tok0:tok0 + 128, :], src, reads=[Bsrc])]
            elif l == 0:
                W.update({"wg": ffn_wg, "wu": ffn_wu, "wd": ffn_wd})

                def out_dma(tok0, src, Bsrc):
                    ops = [p.dma("sp", x1[256 + tok0:256 + tok0 + 128, :], src, reads=[Bsrc])]
                    if tok0 < 256:
                        ops.append(p.dma("sp", edge_own.ap()[tok0:tok0 + 128, :], src, reads=[Bsrc]))
                    if tok0 >= T - 256:
                        e0 = 256 + tok0 - (T - 256)
                        ops.append(p.dma("sp", edge_own.ap()[e0:e0 + 128, :], src, reads=[Bsrc]))
                    return ops
            else:
                W.update({"wg": moe_wg, "wu": moe_wu, "wd": moe_wd, "wr": wr_d})

                def out_dma(tok0, src, Bsrc):
                    return [p.dma("sp", xout[tok0:tok0 + 128, :], src, reads=[Bsrc])]
            k.phase()
            outs = emit_layer(k, nc, l, xl, zall, W, out_dma)
            if l == _DBG_L - 1:
                final = outs
        p.emit(final_wait_ops=final)
    return nc


def build_unfused_a():
    nc = bass.Bass("TRN2", target_bir_lowering=False)
    x = nc.dram_tensor("x", [T, D], F32, kind="ExternalInput").ap()
    g = nc.dram_tensor("g", [D], F32, kind="ExternalInput").ap()
    wf = nc.dram_tensor("wf", [D, 512], F32, kind="ExternalInput").ap()
    cs = nc.dram_tensor("cs", [128, 256], F32, kind="ExternalInput").ap()
    z = nc.dram_tensor("z", [4, T, 256], BF16, kind="ExternalOutput").ap()
    with ExitStack() as st:
        k = K(nc, st)
        k.consts()
        fin = phase_fin(k, lambda i: x[i * 128:(i + 1) * 128, :], g, lambda c0, n: wf[:, c0:c0 + n], cs, z, own_off=0,
                        do_norm=True, hT=None, Bh=None)
        k.p.emit(final_wait_ops=fin)
    return nc


def build_unfused_b(layer):
    moe = (layer % 2 == 1)
    nc = bass.Bass("TRN2", target_bir_lowering=False)
    din = lambda name, shape, dt=F32: nc.dram_tensor(name, shape, dt, kind="ExternalInput").ap()
    x = din("x", [TE, D])
    zall = din("zall", [NCORES, 4, T, 256], BF16)
    W = {"mem": din("mem", [256, D]), "ln_mix_g": din("ln_mix_g", [D]), "w_in": din("w_in", [D, 10240]),
         "na_q_g": din("na_q_g", [128]), "na_k_g": din("na_k_g", [128]), "ttab": din("ttab", [8, 16, 64, 64]),
         "rowmask": din("rowmask", [5, 128, 6, 128]), "mem_ln_g": din("mem_ln_g", [D]),
         "w_mem_kv": din("w_mem_kv", [D, 1024]), "mem_q_g": din("mem_q_g", [128]), "mem_k_g": din("mem_k_g", [128]),
         "w_f_out": din("w_f_out", [512, D]), "w_na_out": din("w_na_out", [1024, D]), "w_mem_out": din("w_mem_out", [512, D]),
         "w_o": din("w_o", [D, D]), "ln_ffn_g": din("ln_ffn_g", [D]), "w128": din("w128", [128, 512]),
         "mtab": din("mtab", [128, 128, 2, 16])}
    if moe:
        W.update({"wr": din("moe_router", [D, 8]), "wg": din("moe_w_gate", [8, D, DFF]), "wu": din("moe_w_up", [8, D, DFF]),
                  "wd": din("moe_w_down", [8, DFF, D])})
    else:
        W.update({"wg": din("ffn_w_gate", [1, D, DFF]), "wu": din("ffn_w_up", [1, D, DFF]), "wd": din("ffn_w_down", [1, DFF, D])})
    xout = nc.dram_tensor("xout", [T, D], F32, kind="ExternalOutput").ap()
    dsc = lambda name, shape, dt: nc.dram_tensor(name, shape, dt).ap()
    W.update({"ft_d": dsc("ft_d", [4, 128, T], BF16), "na_d": dsc("na_d", [8, 128, T], BF16), "mo_d": dsc("mo_d", [4, 128, T], BF16),
              "gates_d": dsc("gates_d", [48, 128, T], F32), "xmid_d": dsc("xmid_d", [T, D], F32)})
    with ExitStack() as st:
        k = K(nc, st)
        k.consts()
        p = k.p

        def out_dma(tok0, src, Bsrc):
            return [p.dma("sp", xout[tok0:tok0 + 128, :], src, reads=[Bsrc])]
        outs = emit_layer(k, nc, layer, x, zall, W, out_dma)
        p.emit(final_wait_ops=outs)
    return nc


def kernel_unfused(inp):
    x = np.ascontiguousarray(inp["x"][0])
    mem = np.ascontiguousarray(inp["mem"][0])
    cs, w128, mtabs = _dft_consts()
    rms = [_rowmask(c) for c in range(NCORES)]
    cores = list(range(NCORES))
    C = np.ascontiguousarray
    for l in range(2):
        if "a" not in _CACHE:
            _CACHE["a"] = build_unfused_a()
        g = C(inp["ln_mix_g"][l])
        ins = [{"x": C(x[c * T:(c + 1) * T]), "g": g, "wf": C(inp["w_in"][l][:, :512]), "cs": cs} for c in cores]
        res = run_bass_kernel_spmd(_CACHE["a"], ins, core_ids=cores)
        zall = np.stack([np.asarray(r["z"]) for r in res.results], axis=0)
        if ("b", l) not in _CACHE:
            _CACHE[("b", l)] = build_unfused_b(l)
        xp = np.concatenate([np.zeros((256, D), np.float32), x, np.zeros((256, D), np.float32)], axis=0)
        common = {"zall": zall, "mem": mem, "ln_mix_g": g, "w_in": C(inp["w_in"][l]), "na_q_g": C(inp["na_q_g"][l]),
                  "na_k_g": C(inp["na_k_g"][l]), "ttab": _na_tables(inp["na_rpb"][l]), "mem_ln_g": C(inp["mem_ln_g"][l]),
                  "w_mem_kv": C(inp["w_mem_kv"][l]), "mem_q_g": C(inp["mem_q_g"][l]), "mem_k_g": C(inp["mem_k_g"][l]),
                  "w_f_out": C(inp["w_fourier_out"][l]), "w_na_out": C(inp["w_na_out"][l]), "w_mem_out": C(inp["w_mem_out"][l]),
                  "w_o": C(inp["w_o"][l]), "ln_ffn_g": C(inp["ln_ffn_g"][l]), "w128": w128}
        i = l // 2
        if l % 2 == 1:
            common.update({"moe_router": C(inp["moe_router"][i]), "moe_w_gate": inp["moe_w_gate"][i],
                           "moe_w_up": inp["moe_w_up"][i], "moe_w_down": inp["moe_w_down"][i]})
        else:
            common.update({"ffn_w_gate": inp["ffn_w_gate"][i:i + 1], "ffn_w_up": inp["ffn_w_up"][i:i + 1],
                           "ffn_w_down": inp["ffn_w_down"][i:i + 1]})
        ins = []
        for c in cores:
            d = dict(common)
            d["x"] = C(xp[c * T:c * T + TE])
            d["rowmask"] = rms[c]
            d["mtab"] = mtabs[c]
            ins.append(d)
        res = run_bass_kernel_spmd(_CACHE[("b", l)], ins, core_ids=cores)
        x = np.concatenate([np.asarray(r["xout"]) for r in res.results], axis=0)
    return x.reshape(1, NCORES * T, D).astype(np.float32)


def kernel(**inp):
    inp = {k_: np.asarray(v) for k_, v in inp.items()}
    if not FUSED:
        return kernel_unfused(inp)
    x = inp["x"][0]
    cs, w128, mtabs = _dft_consts()
    cores = list(range(NCORES))
    if "nc" not in _CACHE:
        _CACHE["nc"] = build_fused()
    nc = _CACHE["nc"]
    xp = np.concatenate([np.zeros((256, D), np.float32), x, np.zeros((256, D), np.float32)], axis=0)
    ttab = np.stack([_na_tables(inp["na_rpb"][l]) for l in range(2)], axis=0)
    common = {k_: inp[k_] for k_ in ("ln_mix_g", "w_in", "na_q_g", "na_k_g", "mem_ln_g", "w_mem_kv", "mem_q_g", "mem_k_g",
                                     "w_fourier_out", "w_na_out", "w_mem_out", "w_o", "ln_ffn_g", "ffn_w_gate", "ffn_w_up",
                                     "ffn_w_down")}
    common.update({"mem": inp["mem"][0], "ttab": ttab, "cs": cs, "w128": w128, "moe_router": inp["moe_router"][0],
                   "moe_w_gate": inp["moe_w_gate"][0], "moe_w_up": inp["moe_w_up"][0], "moe_w_down": inp["moe_w_down"][0]})
    ins = []
    r = np.arange(128)
    for c in cores:
        d = dict(common)
        d["x"] = np.ascontiguousarray(xp[c * T:c * T + TE])
        d["rowmask"] = _rowmask(c)
        d["mtab"] = mtabs[c]
        up = c - 1 if c > 0 else c
        dn = c + 1 if c < NCORES - 1 else c
        hidx = np.stack([up * 512 + 256 + r, up * 512 + 384 + r, dn * 512 + r, dn * 512 + 128 + r], axis=1)
        d["hidx"] = hidx.astype(np.int32)
        ins.append(d)
    res = run_bass_kernel_spmd(nc, ins, core_ids=cores)
    out = np.concatenate([np.asarray(r_["xout"]) for r_ in res.results], axis=0)
    return out.reshape(1, NCORES * T, D).astype(np.float32)
```
